# Optimizing a Trainium2 kernel written in Bass

```python
import math
import jax, jax.numpy as jnp
from jax import lax
import numpy as np


D_MODEL = 2048
BATCH = 2
SEQ = 16384
DEPTH = 4

N_BRANCH = 4
BRANCH_WIDTH = 512
HEAD_DIM = 128
DSA_HEADS = 4
IDX_HEADS = 8
IDX_DIM = 64
TOPK_MAX = 256
S5_WIDTH = 512
S5_GROUP = 16
S5_GROUPS = S5_WIDTH // S5_GROUP
S5_STATE = 64
S5_DT_MIN = 1e-3
S5_DT_MAX = 1e-1
DIFF_HEADS = 4
DIFF_QK_DIM = 64
DIFF_V_DIM = 128
MLSTM_HEADS = 4
MLSTM_DIM = 128
MLSTM_CHUNK = 64
CONV_WIDTH = 4
D_FF = 5504
REL_BUCKETS = 32
REL_MAX_DIST = 128
N_BIAS_HEADS = DSA_HEADS + DIFF_HEADS
Q_BLOCK = 128
EPS = 1e-6

IN_SPLITS = (
    DSA_HEADS * HEAD_DIM,
    DSA_HEADS * HEAD_DIM,
    DSA_HEADS * HEAD_DIM,
    IDX_HEADS * IDX_DIM,
    IDX_DIM,
    IDX_HEADS,
    S5_WIDTH,
    DIFF_HEADS * 2 * DIFF_QK_DIM,
    DIFF_HEADS * 2 * DIFF_QK_DIM,
    DIFF_HEADS * DIFF_V_DIM,
    2 * MLSTM_HEADS * MLSTM_DIM,
    MLSTM_HEADS * MLSTM_DIM,
    MLSTM_HEADS * MLSTM_DIM,
    MLSTM_HEADS,
    MLSTM_HEADS,
)
IN_COLS = sum(IN_SPLITS)

kernel_name = 'hybrid_gated_dsa_s5_diff_mlstm_macaron'


def rmsnorm(x, g):
    xf = x.astype(jnp.float32)
    y = xf * lax.rsqrt(jnp.mean(xf * xf, axis=-1, keepdims=True) + EPS)
    return (y * g.astype(jnp.float32)).astype(x.dtype)


def swiglu(x, w_in, w_out):
    g, u = jnp.split(x @ w_in, 2, axis=-1)
    return (jax.nn.silu(g) * u) @ w_out


def rel_bucket(dist):
    max_exact = REL_BUCKETS // 2
    n = jnp.maximum(dist, 0)
    large = max_exact + (jnp.log(jnp.maximum(n, 1).astype(jnp.float32) / max_exact)
                         / math.log(REL_MAX_DIST / max_exact)
                         * (REL_BUCKETS - max_exact)).astype(jnp.int32)
    large = jnp.minimum(large, REL_BUCKETS - 1)
    return jnp.where(n < max_exact, n, large)


def to_blocks(t):
    b, s = t.shape[:2]
    return jnp.moveaxis(t.reshape((b, s // Q_BLOCK, Q_BLOCK) + t.shape[2:]), 1, 0)


def from_blocks(t):
    t = jnp.moveaxis(t, 0, 1)
    return t.reshape((t.shape[0], t.shape[1] * t.shape[2]) + t.shape[3:])


def causal_conv(x, w):
    s = x.shape[1]
    xp = jnp.pad(x, ((0, 0), (CONV_WIDTH - 1, 0), (0, 0)))
    out = xp[:, 0:s] * w[0]
    for j in range(1, CONV_WIDTH):
        out = out + xp[:, j:j + s] * w[j]
    return out


def dsa_attention(q, k, v, iq, ik, iw, rel_a):
    f32 = jnp.float32
    bsz, s, h, dh = q.shape
    topk = min(TOPK_MAX, s // 4)
    spos = jnp.arange(s)
    scale = dh ** -0.5
    idx_scale = (IDX_DIM * IDX_HEADS) ** -0.5

    def block(args):
        qb, iqb, iwb, t0 = args
        tpos = t0 + jnp.arange(Q_BLOCK)
        rel = jax.nn.relu(jnp.einsum('bqhd,bsd->bqhs', iqb, ik).astype(f32))
        score = jnp.einsum('bqh,bqhs->bqs', iwb.astype(f32), rel) * idx_scale
        score = jnp.where(spos[None, None, :] <= tpos[None, :, None], score, -jnp.inf)
        _, sel = lax.top_k(score, topk)
        valid = sel <= tpos[None, :, None]
        kg = jax.vmap(lambda kk, ii: kk[ii])(k, sel)
        vg = jax.vmap(lambda vv, ii: vv[ii])(v, sel)
        logits = jnp.einsum('bqhd,bqkhd->bqhk', qb, kg).astype(f32) * scale
        bias = rel_a[rel_bucket(tpos[None, :, None] - sel)]
        logits = logits + jnp.swapaxes(bias, -1, -2).astype(f32)
        logits = jnp.where(valid[:, :, None, :], logits, -jnp.inf)
        p = jax.nn.softmax(logits, axis=-1)
        return jnp.einsum('bqhk,bqkhd->bqhd', p.astype(v.dtype), vg)

    t0s = jnp.arange(s // Q_BLOCK) * Q_BLOCK
    out = lax.map(block, (to_blocks(q), to_blocks(iq), to_blocks(iw), t0s))
    return from_blocks(out).reshape(bsz, s, h * dh)


def s5_combine(e1, e2):
    a1r, a1i, b1r, b1i = e1
    a2r, a2i, b2r, b2i = e2
    return (a2r * a1r - a2i * a1i, a2r * a1i + a2i * a1r,
            a2r * b1r - a2i * b1i + b2r, a2r * b1i + a2i * b1r + b2i)


def s5_mixer(u, a_re, a_im, log_dt, b_re, b_im, c_re, c_im, d_skip, w_glu):
    f32 = jnp.float32
    bsz, s, _ = u.shape
    uf = u.astype(f32).reshape(bsz, s, S5_GROUPS, S5_GROUP)
    ar, ai = a_re.astype(f32), a_im.astype(f32)
    dt = jnp.exp(log_dt.astype(f32))[:, None]
    mag = jnp.exp(ar * dt)
    ab_re, ab_im = mag * jnp.cos(ai * dt), mag * jnp.sin(ai * dt)
    den = ar * ar + ai * ai
    nr, ni = ab_re - 1.0, ab_im
    coef_re = (nr * ar + ni * ai) / den
    coef_im = (ni * ar - nr * ai) / den
    br, bi = b_re.astype(f32), b_im.astype(f32)
    bb_re = coef_re[..., None] * br - coef_im[..., None] * bi
    bb_im = coef_re[..., None] * bi + coef_im[..., None] * br
    bu_re = jnp.einsum('gpc,bsgc->bsgp', bb_re, uf)
    bu_im = jnp.einsum('gpc,bsgc->bsgp', bb_im, uf)
    a_full_re = jnp.broadcast_to(ab_re, bu_re.shape)
    a_full_im = jnp.broadcast_to(ab_im, bu_re.shape)
    _, _, x_re, x_im = lax.associative_scan(s5_combine, (a_full_re, a_full_im, bu_re, bu_im), axis=1)
    y = (jnp.einsum('gcp,bsgp->bsgc', c_re.astype(f32), x_re)
         - jnp.einsum('gcp,bsgp->bsgc', c_im.astype(f32), x_im))
    y = y.reshape(bsz, s, S5_WIDTH) + d_skip.astype(f32) * u.astype(f32)
    z = jax.nn.gelu(y).astype(u.dtype)
    return z * jax.nn.sigmoid(z @ w_glu)


def diff_attention(q, k, v, rel_c, lam, lambda_init, subln_g):
    f32 = jnp.float32
    bsz, s, h, _, dq = q.shape
    scale = dq ** -0.5
    spos = jnp.arange(s)

    def block(args):
        qb, t0 = args
        tpos = t0 + jnp.arange(Q_BLOCK)
        logits = jnp.einsum('bqhmd,bshmd->bhmqs', qb, k).astype(f32) * scale
        bias = rel_c[rel_bucket(tpos[:, None] - spos[None, :])]
        logits = logits + jnp.transpose(bias, (2, 0, 1))[None, :, None].astype(f32)
        logits = jnp.where(spos[None, :] <= tpos[:, None], logits, -jnp.inf)
        p = jax.nn.softmax(logits, axis=-1)
        a = p[:, :, 0] - lam * p[:, :, 1]
        return jnp.einsum('bhqs,bshd->bqhd', a.astype(v.dtype), v)

    t0s = jnp.arange(s // Q_BLOCK) * Q_BLOCK
    out = from_blocks(lax.map(block, (to_blocks(q), t0s)))
    out = rmsnorm(out, subln_g) * (1.0 - lambda_init)
    return out.reshape(bsz, s, h * v.shape[-1])


def mlstm_chunkwise(q, k, v, i_pre, f_pre):
    bsz, s, h, d = q.shape
    nc = s // MLSTM_CHUNK

    def to_chunks(t):
        return t.reshape(bsz, nc, MLSTM_CHUNK, h, -1).transpose(0, 3, 1, 2, 4)

    def gate_chunks(t):
        return t.reshape(bsz, nc, MLSTM_CHUNK, h).transpose(0, 3, 1, 2)

    q, k, v = to_chunks(q), to_chunks(k) * (d ** -0.5), to_chunks(v)
    ig = gate_chunks(i_pre)
    bcum = jnp.cumsum(gate_chunks(jax.nn.log_sigmoid(f_pre)), axis=-1)
    b_last = bcum[..., -1]
    g = b_last[..., None] - bcum + ig
    m_loc = jnp.max(g, axis=-1)
    w = jnp.exp(g - m_loc[..., None])
    c_loc = jnp.einsum('bhcl,bhcld,bhcle->bhcde', w, k, v)
    n_loc = jnp.einsum('bhcl,bhcld->bhcd', w, k)

    def step(carry, inp):
        c_st, n_st, m_st = carry
        bl, ml, cl, nl = inp
        m_new = jnp.maximum(bl + m_st, ml)
        a = jnp.exp(bl + m_st - m_new)
        bcoef = jnp.exp(ml - m_new)
        c_new = a[..., None, None] * c_st + bcoef[..., None, None] * cl
        n_new = a[..., None] * n_st + bcoef[..., None] * nl
        return (c_new, n_new, m_new), (c_st, n_st, m_st)

    init = (jnp.zeros((bsz, h, d, d), jnp.float32), jnp.zeros((bsz, h, d), jnp.float32),
            jnp.zeros((bsz, h), jnp.float32))
    xs = (jnp.moveaxis(b_last, 2, 0), jnp.moveaxis(m_loc, 2, 0),
          jnp.moveaxis(c_loc, 2, 0), jnp.moveaxis(n_loc, 2, 0))
    _, (c_prev, n_prev, m_prev) = lax.scan(step, init, xs)
    c_prev = jnp.moveaxis(c_prev, 0, 2)
    n_prev = jnp.moveaxis(n_prev, 0, 2)
    m_prev = jnp.moveaxis(m_prev, 0, 2)

    causal = jnp.tril(jnp.ones((MLSTM_CHUNK, MLSTM_CHUNK), dtype=bool))
    log_d = bcum[..., :, None] - bcum[..., None, :] + ig[..., None, :]
    log_d = jnp.where(causal, log_d, -jnp.inf)
    m_inter = bcum + m_prev[..., None]
    m_j = jnp.maximum(jnp.max(log_d, axis=-1), m_inter)
    dqk = jnp.exp(log_d - m_j[..., None]) * jnp.einsum('bhcjd,bhcsd->bhcjs', q, k)
    inter = jnp.exp(m_inter - m_j)
    num = (jnp.einsum('bhcjs,bhcse->bhcje', dqk, v)
           + inter[..., None] * jnp.einsum('bhcjd,bhcde->bhcje', q, c_prev))
    den = jnp.sum(dqk, axis=-1) + inter * jnp.einsum('bhcjd,bhcd->bhcj', q, n_prev)
    hid = num / jnp.maximum(jnp.abs(den), jnp.exp(-m_j))[..., None]
    return hid.transpose(0, 2, 3, 1, 4).reshape(bsz, s, h * d)


def hybrid_mixer(xn, w_in, conv_w, i_bias, f_bias, s5_a_re, s5_a_im, s5_log_dt, s5_b_re, s5_b_im,
                 s5_c_re, s5_c_im, s5_d, s5_w_glu, diff_lambda, diff_subln_g, lambda_init,
                 rel_table, w_gate, b_gate, w_branch, w_out):
    f32 = jnp.float32
    bsz, s, _ = xn.shape
    h = xn @ w_in
    split_points = np.cumsum(IN_SPLITS)[:-1].tolist()
    (a_q, a_k, a_v, a_iq, a_ik, a_iw, b_u, c_q, c_k, c_v,
     d_qk, d_v, d_o, d_i, d_f) = jnp.split(h, split_points, axis=-1)

    y_a = dsa_attention(a_q.reshape(bsz, s, DSA_HEADS, HEAD_DIM), a_k.reshape(bsz, s, DSA_HEADS, HEAD_DIM),
                        a_v.reshape(bsz, s, DSA_HEADS, HEAD_DIM), a_iq.reshape(bsz, s, IDX_HEADS, IDX_DIM),
                        a_ik, a_iw, rel_table[:, :DSA_HEADS])
    y_b = s5_mixer(b_u, s5_a_re, s5_a_im, s5_log_dt, s5_b_re, s5_b_im, s5_c_re, s5_c_im, s5_d, s5_w_glu)
    dl = diff_lambda.astype(f32)
    lam = jnp.exp(jnp.sum(dl[0] * dl[1])) - jnp.exp(jnp.sum(dl[2] * dl[3])) + lambda_init
    y_c = diff_attention(c_q.reshape(bsz, s, DIFF_HEADS, 2, DIFF_QK_DIM),
                         c_k.reshape(bsz, s, DIFF_HEADS, 2, DIFF_QK_DIM),
                         c_v.reshape(bsz, s, DIFF_HEADS, DIFF_V_DIM),
                         rel_table[:, DSA_HEADS:], lam, lambda_init, diff_subln_g)
    qk = jax.nn.silu(causal_conv(d_qk, conv_w))
    m_q, m_k = jnp.split(qk.astype(f32), 2, axis=-1)
    hm = mlstm_chunkwise(m_q.reshape(bsz, s, MLSTM_HEADS, MLSTM_DIM), m_k.reshape(bsz, s, MLSTM_HEADS, MLSTM_DIM),
                         d_v.astype(f32).reshape(bsz, s, MLSTM_HEADS, MLSTM_DIM),
                         d_i.astype(f32) + i_bias.astype(f32), d_f.astype(f32) + f_bias.astype(f32))
    y_d = (jax.nn.sigmoid(d_o.astype(f32)) * hm).astype(xn.dtype)

    ys = (y_a, y_b, y_c, y_d)
    merged = jax.nn.sigmoid(xn @ w_gate[0] + b_gate[0]) * (ys[0] @ w_branch[0])
    for n in range(1, N_BRANCH):
        merged = merged + jax.nn.sigmoid(xn @ w_gate[n] + b_gate[n]) * (ys[n] @ w_branch[n])
    return merged @ w_out


def setup_inputs(seed: int = 0) -> dict:
    key = jax.random.key(seed)
    ks = jax.random.split(key, 26)
    f32 = jnp.float32

    def nrm(k, shape, scale):
        return jax.random.normal(k, shape, f32) * scale

    return {
        'x': nrm(ks[0], (BATCH, SEQ, D_MODEL), 1.0),
        'norm_g': 1.0 + nrm(ks[1], (DEPTH, 3, D_MODEL), 0.02),
        'w_ffn_in': nrm(ks[2], (DEPTH, 2, D_MODEL, 2 * D_FF), D_MODEL ** -0.5),
        'w_ffn_out': nrm(ks[3], (DEPTH, 2, D_FF, D_MODEL), D_FF ** -0.5),
        'w_in': nrm(ks[4], (DEPTH, D_MODEL, IN_COLS), D_MODEL ** -0.5),
        'conv_w': nrm(ks[5], (DEPTH, CONV_WIDTH, 2 * MLSTM_HEADS * MLSTM_DIM), CONV_WIDTH ** -0.5),
        'mlstm_i_bias': nrm(ks[6], (DEPTH, MLSTM_HEADS), 0.1),
        'mlstm_f_bias': jnp.linspace(3.0, 6.0, MLSTM_HEADS, dtype=f32)[None] + nrm(ks[7], (DEPTH, MLSTM_HEADS), 0.1),
        's5_a_re': -0.5 + nrm(ks[8], (DEPTH, S5_GROUPS, S5_STATE), 0.01),
        's5_a_im': math.pi * jnp.arange(S5_STATE, dtype=f32) + nrm(ks[9], (DEPTH, S5_GROUPS, S5_STATE), 0.01),
        's5_log_dt': jax.random.uniform(ks[10], (DEPTH, S5_GROUPS), f32, math.log(S5_DT_MIN), math.log(S5_DT_MAX)),
        's5_b_re': nrm(ks[11], (DEPTH, S5_GROUPS, S5_STATE, S5_GROUP), (2 * S5_GROUP) ** -0.5),
        's5_b_im': nrm(ks[12], (DEPTH, S5_GROUPS, S5_STATE, S5_GROUP), (2 * S5_GROUP) ** -0.5),
        's5_c_re': nrm(ks[13], (DEPTH, S5_GROUPS, S5_GROUP, S5_STATE), S5_STATE ** -0.5),
        's5_c_im': nrm(ks[14], (DEPTH, S5_GROUPS, S5_GROUP, S5_STATE), S5_STATE ** -0.5),
        's5_d': nrm(ks[15], (DEPTH, S5_WIDTH), 1.0),
        's5_w_glu': nrm(ks[16], (DEPTH, S5_WIDTH, S5_WIDTH), S5_WIDTH ** -0.5),
        'diff_lambda': nrm(ks[17], (DEPTH, 4, DIFF_QK_DIM), 0.1),
        'diff_subln_g': 1.0 + nrm(ks[18], (DEPTH, DIFF_V_DIM), 0.02),
        'rel_table': nrm(ks[19], (REL_BUCKETS, N_BIAS_HEADS), 0.5),
        'w_gate': nrm(ks[20], (DEPTH, N_BRANCH, D_MODEL, D_MODEL), D_MODEL ** -0.5),
        'b_gate': nrm(ks[21], (DEPTH, N_BRANCH, D_MODEL), 0.02),
        'w_branch': nrm(ks[22], (DEPTH, N_BRANCH, BRANCH_WIDTH, D_MODEL), BRANCH_WIDTH ** -0.5),
        'w_out': nrm(ks[23], (DEPTH, D_MODEL, D_MODEL), D_MODEL ** -0.5),
        'final_g': 1.0 + nrm(ks[24], (D_MODEL,), 0.02),
    }


def reference(x, norm_g, w_ffn_in, w_ffn_out, w_in, conv_w, mlstm_i_bias, mlstm_f_bias,
              s5_a_re, s5_a_im, s5_log_dt, s5_b_re, s5_b_im, s5_c_re, s5_c_im, s5_d, s5_w_glu,
              diff_lambda, diff_subln_g, rel_table, w_gate, b_gate, w_branch, w_out, final_g):
    for l in range(DEPTH):
        lambda_init = 0.8 - 0.6 * math.exp(-0.3 * l)
        x = x + 0.5 * swiglu(rmsnorm(x, norm_g[l, 0]), w_ffn_in[l, 0], w_ffn_out[l, 0])
        x = x + hybrid_mixer(rmsnorm(x, norm_g[l, 1]), w_in[l], conv_w[l], mlstm_i_bias[l], mlstm_f_bias[l],
                             s5_a_re[l], s5_a_im[l], s5_log_dt[l], s5_b_re[l], s5_b_im[l],
                             s5_c_re[l], s5_c_im[l], s5_d[l], s5_w_glu[l], diff_lambda[l], diff_subln_g[l],
                             lambda_init, rel_table, w_gate[l], b_gate[l], w_branch[l], w_out[l])
        x = x + 0.5 * swiglu(rmsnorm(x, norm_g[l, 2]), w_ffn_in[l, 1], w_ffn_out[l, 1])
    return rmsnorm(x, final_g)
```

```python
import math
from contextlib import ExitStack
import numpy as np
import ml_dtypes
import concourse.bass as bass
import concourse.mybir as mybir
from concourse.bass_utils import run_bass_kernel_spmd

F32 = mybir.dt.float32
BF16 = mybir.dt.bfloat16
AF = mybir.ActivationFunctionType
ALU = mybir.AluOpType
AX = mybir.AxisListType

D = 2048
DFF = 5504
EPS = 1e-6
NF = 4864
NT = 1544
O_AQ, O_AK, O_IQ, O_IK, O_BU, O_CQ, O_CK, O_DQK, O_DO, O_G = 0, 512, 1024, 1536, 1664, 2176, 2688, 3200, 4224, 4736
NFU = 4736
NITER = 22


class Buf:
    __slots__ = ("t", "w", "r", "name")

    def __init__(self, t, name=""):
        self.t = t
        self.w = None
        self.r = {}
        self.name = name

    def __getitem__(self, k):
        return self.t[k]


class Ctx:
    NDMA = 24

    def __init__(self, nc, es):
        self.nc = nc
        self.es = es
        self.eng = {"pe": nc.tensor, "act": nc.scalar, "dve": nc.vector,
                    "pool": nc.gpsimd, "sp": nc.sync}
        self.sem = {}
        self.cnt = {}
        for e in self.eng:
            self.sem[e] = es.enter_context(nc.semaphore("s_" + e))
            self.cnt[e] = 0
        for i in range(self.NDMA):
            k = "d%d" % i
            self.sem[k] = es.enter_context(nc.semaphore("s_" + k))
            self.cnt[k] = 0
        self.seen = {e: {} for e in self.eng}
        self.dma_rr = 0
        self.nins = 0
        self.uid = 0

    def name(self, n):
        self.uid += 1
        return "%s_%d" % (n, self.uid)

    def sb(self, shape, dt, name, es=None):
        es = es or self.es
        return Buf(es.enter_context(self.nc.sbuf_tensor(self.name(name), list(shape), dt)), name)

    def ps(self, shape, dt, name, es=None):
        es = es or self.es
        return Buf(es.enter_context(self.nc.psum_tensor(self.name(name), list(shape), dt)), name)

    def dram(self, shape, dt, name, kind="Internal"):
        return Buf(self.nc.dram_tensor(name, list(shape), dt, kind=kind).ap(), name)

    def _wait(self, e, deps):
        for (k, v) in sorted(deps):
            if k == e and e == "pe":
                continue
            if self.seen[e].get(k, 0) < v:
                self.eng[e].wait_ge(self.sem[k], v)
                self.seen[e][k] = v

    def _deps(self, reads, writes):
        deps = set()
        for b in reads:
            if b.w is not None:
                deps.add(b.w)
        for b in writes:
            if b.w is not None:
                deps.add(b.w)
            for kv in b.r.items():
                deps.add(kv)
        return deps

    def op(self, e, fn, reads=(), writes=()):
        self._wait(e, self._deps(reads, writes))
        ins = fn(self.eng[e])
        self.cnt[e] += 1
        ins.then_inc(self.sem[e], 1)
        me = (e, self.cnt[e])
        for b in reads:
            b.r[e] = self.cnt[e]
        for b in writes:
            b.w = me
            b.r = {}
        self.nins += 1
        return ins

    def dma(self, q, out, in_, reads=(), writes=(), **kw):
        k = "d%d" % self.dma_rr
        self.dma_rr = (self.dma_rr + 1) % self.NDMA
        deps = self._deps(reads, writes)
        if self.cnt[k] > 0:
            deps.add((k, self.cnt[k]))
        self._wait(q, deps)
        ins = self.eng[q].dma_start(out=out, in_=in_, **kw)
        self.cnt[k] += 16
        ins.then_inc(self.sem[k], 16)
        me = (k, self.cnt[k])
        for b in reads:
            b.r[k] = self.cnt[k]
        for b in writes:
            b.w = me
            b.r = {}
        self.nins += 1
        return ins

    def barrier(self):
        allk = set((k, v) for k, v in self.cnt.items() if v > 0)
        for e in self.eng:
            self._wait(e, allk)

    def finish(self):
        self._wait("sp", set((k, v) for k, v in self.cnt.items() if v > 0))


class Rot:
    def __init__(self, bufs):
        self.bufs = bufs
        self.i = 0

    def get(self):
        b = self.bufs[self.i]
        self.i = (self.i + 1) % len(self.bufs)
        return b


def build(S, DEPTH, dbg=None, mode="full"):
    DD = max(DEPTH, 1)
    nc = bass.Bass("TRN2", target_bir_lowering=False)
    NTT = S // 512
    NB = S // 128

    def din(name, shape, dt=F32):
        return nc.dram_tensor(name, list(shape), dt, kind="ExternalInput").ap()

    xT_in = din("xT", [D, S])
    normg = din("normg", [DD, 3, 128, 16])
    finalg = din("finalg", [128, 16])
    wffi = [din("wffi%d" % l, [2, D, 2 * DFF]) for l in range(DEPTH)]
    wffo = [din("wffo%d" % l, [2, DFF, D]) for l in range(DEPTH)]
    wfm = [din("wfm%d" % l, [D, NF]) for l in range(DEPTH)]
    wtm = [din("wtm%d" % l, [D, NT]) for l in range(DEPTH)]
    wgate = [din("wgate%d" % l, [4, D, D]) for l in range(DEPTH)]
    bgate = din("bgate", [DD, 4, 128, 16])
    wbr = [din("wbr%d" % l, [4, 512, D]) for l in range(DEPTH)]
    wout = [din("wout%d" % l, [D, D]) for l in range(DEPTH)]
    ident_in = din("ident", [128, 128])
    nbias = din("nbias", [8, 128, 256])
    c31_in = din("c31", [128, 8])
    cneg_in = din("cneg", [128, 128])
    cpos_in = din("cpos", [128, 128])
    p2tab_in = din("p2tab", [128, NITER])
    sublng = din("sublng", [DD, 128, 128])
    dlam = din("dlam", [DD, 128, 256])
    s5ar = din("s5ar", [DD, 128, 16])
    s5ai = din("s5ai", [DD, 128, 16])
    s5ldt = din("s5ldt", [DD, 128, 16])
    s5b = din("s5b", [DD, 32, 128, 128])
    s5c = din("s5c", [DD, 32, 128, 128])
    s5d = din("s5d", [DD, 128, 4])
    s5wglu = din("s5wglu", [DD, 512, 512])
    iota_in = din("iota", [128, 512])
    convw = din("convw", [DD, 128, 8, 4])
    mbias = din("mbias", [DD, 128, 8])
    ctri_in = din("ctri", [128, 128])
    lamc_in = din("lamc", [DD, 128, 2])
    dbg_y = nc.dram_tensor("dbgy", [D, S], BF16, kind="ExternalOutput").ap() if dbg is not None else None
    MIX = dbg if dbg is not None else "abcd"
    out_T = nc.dram_tensor("outT", [D, S], F32, kind="ExternalOutput").ap()

    with ExitStack() as es:
        c = Ctx(nc, es)
        xT = c.dram([D, S], F32, "xTs")
        hT = c.dram([NFU, S], BF16, "hT")
        giT = c.dram([128, S], F32, "giT")
        vtm = c.dram([S, 1536], BF16, "vtm")
        iwt = c.dram([S, 8], F32, "iwt")
        yT = c.dram([D, S], BF16, "yT")
        outb = Buf(out_T, "out")
        xin = Buf(xT_in, "xin")
        wdummy = Buf(None, "w")

        ones_f = c.sb([128, 128], F32, "ones_f")
        ident_f = c.sb([128, 128], F32, "ident_f")
        ident_b = c.sb([128, 128], BF16, "ident_b")
        gcol = c.sb([128, DD * 3 + 1, 16], F32, "gcol")
        bgc = c.sb([128, DD * 4, 16], F32, "bgc")
        lamc = c.sb([128, DD, 2], F32, "lamc")
        for l in range(DD):
            c.dma("sp", lamc[:, l, :], lamc_in[l], writes=[lamc])
        c.op("dve", lambda e: e.memset(ones_f[:], 1.0), writes=[ones_f])
        c.dma("sp", ident_f[:], ident_in, writes=[ident_f])
        c.op("dve", lambda e: e.tensor_copy(out=ident_b[:], in_=ident_f[:]), reads=[ident_f], writes=[ident_b])
        for l in range(DEPTH):
            for j in range(3):
                c.dma("sp", gcol[:, l * 3 + j, :], normg[l, j], writes=[gcol])
            for n in range(4):
                c.dma("sp", bgc[:, l * 4 + n, :], bgate[l, n], writes=[bgc])
        c.dma("sp", gcol[:, DEPTH * 3, :], finalg, writes=[gcol])
        c31 = c.sb([128, 8], F32, "c31")
        cneg = c.sb([128, 128], F32, "cneg")
        cpos = c.sb([128, 128], F32, "cpos")
        p2tab = c.sb([128, NITER], F32, "p2tab")
        c.dma("sp", c31[:], c31_in, writes=[c31])
        c.dma("sp", cneg[:], cneg_in, writes=[cneg])
        c.dma("sp", cpos[:], cpos_in, writes=[cpos])
        c.dma("sp", p2tab[:], p2tab_in, writes=[p2tab])
        ctri = c.sb([128, 128], F32, "ctri")
        c.dma("sp", ctri[:], ctri_in, writes=[ctri])
        small = Rot([c.sb([128, 8], F32, "small%d" % i) for i in range(4)])
        obuf = Rot([c.sb([128, 128], BF16, "obuf%d" % i) for i in range(4)])

        def token_phase(stage):
            with ExitStack() as es2:
                xt = c.sb([128, 16, 512], F32, "xt", es2)
                xn = c.sb([128, 16, 512], BF16, "xn", es2)
                big = c.sb([128, 43 * 512], BF16, "big", es2)
                aT = big.t[:].rearrange("p (k t) -> p k t", t=512)
                mg = big.t[:, 0:16 * 512 * 2].bitcast(F32).rearrange("p (k t) -> p k t", t=512)
                wts = Rot([c.sb([128, 16, 512], BF16, "wt%d" % i, es2) for i in range(3)])
                wos = Rot([c.sb([128, 1024], BF16, "wo%d" % i, es2) for i in range(4)])
                tmpf = Rot([c.sb([128, 512], F32, "tmpf%d" % i, es2) for i in range(3)])
                stg = Rot([c.sb([128, 512], BF16, "stg%d" % i, es2) for i in range(3)])
                rstd = c.sb([128, 512], F32, "rstd", es2)
                ytl = c.sb([128, 16, 512], BF16, "ytl", es2)
                pss = Rot([c.ps([128, 512], F32, "psA%d" % i, es2) for i in range(8)])

                def norm(gi):
                    ps = pss.get()
                    for kc in range(16):
                        sq = tmpf.get()
                        c.op("act", lambda e: e.activation(out=sq[:], in_=xt[:, kc, :], func=AF.Square),
                             reads=[xt], writes=[sq])
                        c.op("pe", lambda e: e.matmul(ps[:], ones_f[:], sq[:], start=(kc == 0), stop=(kc == 15)),
                             reads=[sq, ones_f], writes=[ps])
                    c.op("act", lambda e: e.activation(out=rstd[:], in_=ps[:], func=AF.Sqrt, scale=1.0 / D, bias=EPS),
                         reads=[ps], writes=[rstd])
                    c.op("dve", lambda e: e.reciprocal(out=rstd[:], in_=rstd[:]), reads=[rstd], writes=[rstd])
                    for kc in range(16):
                        c.op("dve", lambda e: e.scalar_tensor_tensor(
                            out=xn[:, kc, :], in0=xt[:, kc, :], scalar=gcol[:, gi, kc:kc + 1], in1=rstd[:],
                            op0=ALU.mult, op1=ALU.mult), reads=[xt, gcol, rstd], writes=[xn])

                def load_w(wap, j0, w, kchunks=16):
                    wt = wts.get()
                    c.dma("pool", wt[:, 0:kchunks, 0:w], wap[:, j0:j0 + w].rearrange("(kc p) n -> p kc n", p=128),
                          writes=[wt])
                    return wt

                def mm_fm(ps, wt, jc, src, kchunks=16, koff=0):
                    for kc in range(kchunks):
                        c.op("pe", lambda e: e.matmul(ps[:], wt[:, kc, jc * 128:(jc + 1) * 128], src[:, koff + kc, :],
                                                      start=(kc == 0), stop=(kc == kchunks - 1)),
                             reads=[wt, src], writes=[ps])

                def ffn(l, which, gi):
                    norm(gi)
                    wi = wffi[l][which]
                    wo_ap = wffo[l][which]
                    for j0 in range(0, DFF, 512):
                        w = min(512, DFF - j0)
                        wg = load_w(wi, j0, w)
                        wu = load_w(wi, DFF + j0, w)
                        for jc in range(w // 128):
                            pg = pss.get()
                            pu = pss.get()
                            mm_fm(pg, wg, jc, xn)
                            mm_fm(pu, wu, jc, xn)
                            sg = tmpf.get()
                            c.op("act", lambda e: e.activation(out=sg[:], in_=pg[:], func=AF.Silu), reads=[pg], writes=[sg])
                            ch = j0 // 128 + jc
                            c.op("dve", lambda e: e.tensor_tensor(out=aT[:, ch, :], in0=sg[:], in1=pu[:], op=ALU.mult),
                                 reads=[sg, pu], writes=[big])
                    for half in range(2):
                        accs = [pss.get() for _ in range(8)]
                        for kc in range(43):
                            wo = wos.get()
                            c.dma("pool", wo[:], wo_ap[kc * 128:(kc + 1) * 128, half * 1024:(half + 1) * 1024], writes=[wo])
                            for j in range(8):
                                c.op("pe", lambda e: e.matmul(accs[j][:], wo[:, j * 128:(j + 1) * 128], aT[:, kc, :],
                                                              start=(kc == 0), stop=(kc == 42)),
                                     reads=[wo, big], writes=[accs[j]])
                        for j in range(8):
                            oc = half * 8 + j
                            c.op("dve", lambda e: e.scalar_tensor_tensor(
                                out=xt[:, oc, :], in0=accs[j][:], scalar=0.5, in1=xt[:, oc, :], op0=ALU.mult, op1=ALU.add),
                                reads=[accs[j], xt], writes=[xt])

                for tt in range(NTT):
                    tsl = slice(tt * 512, (tt + 1) * 512)
                    if stage == 0:
                        c.dma("sp", xt[:], xT_in[:, tsl].rearrange("(kc p) t -> p kc t", p=128), reads=[xin], writes=[xt])
                    else:
                        c.dma("sp", xt[:], xT[:, tsl].rearrange("(kc p) t -> p kc t", p=128), reads=[xT], writes=[xt])
                    if stage > 0:
                        l = stage - 1
                        norm(l * 3 + 1)
                        c.dma("sp", ytl[:], yT[:, tsl].rearrange("(kc p) t -> p kc t", p=128), reads=[yT], writes=[ytl])
                        for n in range(4):
                            for j0 in range(0, D, 512):
                                wg = load_w(wgate[l][n], j0, 512)
                                wb = load_w(wbr[l][n], j0, 512, kchunks=4)
                                for jc in range(4):
                                    oc = j0 // 128 + jc
                                    pg = pss.get()
                                    pb = pss.get()
                                    mm_fm(pg, wg, jc, xn)
                                    mm_fm(pb, wb, jc, ytl, kchunks=4, koff=n * 4)
                                    sg = tmpf.get()
                                    c.op("act", lambda e: e.activation(out=sg[:], in_=pg[:], func=AF.Sigmoid,
                                                                       bias=bgc[:, l * 4 + n, oc:oc + 1]),
                                         reads=[pg, bgc], writes=[sg])
                                    if n == 0:
                                        c.op("dve", lambda e: e.tensor_tensor(out=mg[:, oc, :], in0=sg[:], in1=pb[:], op=ALU.mult),
                                             reads=[sg, pb], writes=[big])
                                    else:
                                        sg2 = tmpf.get()
                                        c.op("dve", lambda e: e.tensor_tensor(out=sg2[:], in0=sg[:], in1=pb[:], op=ALU.mult),
                                             reads=[sg, pb], writes=[sg2])
                                        c.op("dve", lambda e: e.tensor_tensor(out=mg[:, oc, :], in0=mg[:, oc, :], in1=sg2[:], op=ALU.add),
                                             reads=[sg2, big], writes=[big])
                        for kc in range(16):
                            c.op("act", lambda e: e.copy(out=xn[:, kc, :], in_=mg[:, kc, :]), reads=[big], writes=[xn])
                        for j0 in range(0, D, 512):
                            wo_ = load_w(wout[l], j0, 512)
                            for jc in range(4):
                                oc = j0 // 128 + jc
                                ps = pss.get()
                                mm_fm(ps, wo_, jc, xn)
                                c.op("dve", lambda e: e.tensor_tensor(out=xt[:, oc, :], in0=xt[:, oc, :], in1=ps[:], op=ALU.add),
                                     reads=[ps, xt], writes=[xt])
                        ffn(l, 1, l * 3 + 2)
                    if stage < DEPTH:
                        l = stage
                        ffn(l, 0, l * 3 + 0)
                        c.dma("sp", xT[:, tsl].rearrange("(kc p) t -> p kc t", p=128), xt[:], reads=[xt], writes=[xT])
                        norm(l * 3 + 1)
                        for j0 in range(0, NF, 512):
                            w = min(512, NF - j0)
                            wt = load_w(wfm[l], j0, w)
                            for jc in range(w // 128):
                                ch = j0 // 128 + jc
                                ps = pss.get()
                                mm_fm(ps, wt, jc, xn)
                                if ch * 128 < NFU:
                                    st = stg.get()
                                    c.op("act", lambda e: e.copy(out=st[:], in_=ps[:]), reads=[ps], writes=[st])
                                    c.dma("sp", hT[ch * 128:(ch + 1) * 128, tsl], st[:], reads=[st], writes=[hT])
                                else:
                                    sf = tmpf.get()
                                    c.op("act", lambda e: e.copy(out=sf[:], in_=ps[:]), reads=[ps], writes=[sf])
                                    c.dma("sp", giT[:, tsl], sf[:], reads=[sf], writes=[giT])
                        for j0 in range(0, NT, 512):
                            w = min(512, NT - j0)
                            wt = load_w(wtm[l], j0, w)
                            for tb in range(4):
                                ps = pss.get()
                                for kc in range(16):
                                    c.op("pe", lambda e: e.matmul(ps[:, 0:w], xn[:, kc, tb * 128:(tb + 1) * 128], wt[:, kc, 0:w],
                                                                  start=(kc == 0), stop=(kc == 15)),
                                         reads=[wt, xn], writes=[ps])
                                r0 = tt * 512 + tb * 128
                                if w == 512:
                                    st = stg.get()
                                    c.op("act", lambda e: e.copy(out=st[:], in_=ps[:]), reads=[ps], writes=[st])
                                    c.dma("sp", vtm[r0:r0 + 128, j0:j0 + 512], st[:], reads=[st], writes=[vtm])
                                else:
                                    sf = tmpf.get()
                                    c.op("act", lambda e: e.copy(out=sf[:, 0:w], in_=ps[:, 0:w]), reads=[ps], writes=[sf])
                                    c.dma("sp", iwt[r0:r0 + 128, :], sf[:, 0:w], reads=[sf], writes=[iwt])
                    elif mode == "layer":
                        c.dma("sp", out_T[:, tsl].rearrange("(kc p) t -> p kc t", p=128), xt[:], reads=[xt], writes=[outb])
                    else:
                        ps = pss.get()
                        for kc in range(16):
                            sq = tmpf.get()
                            c.op("act", lambda e: e.activation(out=sq[:], in_=xt[:, kc, :], func=AF.Square),
                                 reads=[xt], writes=[sq])
                            c.op("pe", lambda e: e.matmul(ps[:], ones_f[:], sq[:], start=(kc == 0), stop=(kc == 15)),
                                 reads=[sq, ones_f], writes=[ps])
                        c.op("act", lambda e: e.activation(out=rstd[:], in_=ps[:], func=AF.Sqrt, scale=1.0 / D, bias=EPS),
                             reads=[ps], writes=[rstd])
                        c.op("dve", lambda e: e.reciprocal(out=rstd[:], in_=rstd[:]), reads=[rstd], writes=[rstd])
                        for kc in range(16):
                            c.op("dve", lambda e: e.scalar_tensor_tensor(
                                out=xt[:, kc, :], in0=xt[:, kc, :], scalar=gcol[:, DEPTH * 3, kc:kc + 1], in1=rstd[:],
                                op0=ALU.mult, op1=ALU.mult), reads=[xt, gcol, rstd], writes=[xt])
                        c.dma("sp", out_T[:, tsl].rearrange("(kc p) t -> p kc t", p=128), xt[:], reads=[xt], writes=[outb])
                c.barrier()

        TOPK = min(256, S // 4)
        zT = c.dram([512, S], BF16, "zTs")
        qkc = c.dram([1024, S], BF16, "qkcs")
        NSPL = 4
        RPS = S // NSPL
        maskds = [c.dram([RPS, S], BF16, "maskd%d" % i) for i in range(NSPL)]

        def mrows(tb):
            i = (tb * 128) // RPS
            r0 = tb * 128 - i * RPS
            return maskds[i], r0

        def attention_pass(l, qrow, krow, dq, nmaps, vcol, scale, hb, use_mask, post):
            with ExitStack() as es2:
                kT = c.sb([128, S], BF16, "kT", es2)
                vx = c.sb([128, NB, 129], BF16, "vx", es2)
                qTs = Rot([c.sb([128, 128], BF16, "qT%d" % i, es2) for i in range(2)])
                mks = Rot([c.sb([128, S], BF16, "mk%d" % i, es2) for i in range(2)]) if use_mask else None
                pbs = Rot([c.sb([128, 512], BF16, "pb%d" % i, es2) for i in range(3)])
                tms = Rot([c.sb([128, 256], F32, "tm%d" % i, es2) for i in range(2)])
                pTs = Rot([c.sb([128, 4, 128], BF16, "pTs%d" % i, es2) for i in range(3)])
                nbt = c.sb([128, 256], F32, "nbt", es2)
                pss = Rot([c.ps([128, 512], F32, "psS%d" % i, es2) for i in range(3)])
                pTp = Rot([c.ps([128, 8, 128], BF16, "pTp%d" % i, es2) for i in range(2)])
                pos = Rot([c.ps([128, 512], F32, "po%d" % i, es2) for i in range(2)])
                ptr = c.ps([128, 4, 128], BF16, "ptr", es2)
                c.dma("sp", kT[:], hT[krow:krow + 128, :], reads=[hT], writes=[kT])
                c.op("pool", lambda e: e.memset(vx[:], 1.0), writes=[vx])
                c.dma("sp", vx[:, :, 0:128], vtm[:, vcol:vcol + 128].rearrange("(nb p) d -> p nb d", p=128),
                      reads=[vtm], writes=[vx])
                c.dma("sp", nbt[:], nbias[hb], writes=[nbt])
                for tb in range(NB):
                    L = (tb + 1) * 128
                    Lf = max(0, (tb - 1) * 128)
                    qT = qTs.get()
                    c.dma("sp", qT[:], hT[qrow:qrow + 128, tb * 128:(tb + 1) * 128], reads=[hT], writes=[qT])
                    mk = None
                    if use_mask:
                        mk = mks.get()
                        md, r0 = mrows(tb)
                        c.dma("sp", mk[:, 0:L], md[r0:r0 + 128, 0:L], reads=[md], writes=[mk])
                    tiles = [(j0, min(512, Lf - j0), False) for j0 in range(0, Lf, 512)] + [(Lf, L - Lf, True)]
                    pol = []
                    for m in range(nmaps):
                        po = pos.get()
                        pol.append(po)
                        nblk = L // 128
                        bi = 0
                        for (j0, w, near) in tiles:
                            ps = pss.get()
                            c.op("pe", lambda e: e.matmul(ps[:, 0:w], qT[m * dq:(m + 1) * dq, :], kT[m * dq:(m + 1) * dq, j0:j0 + w],
                                                          start=True, stop=True), reads=[qT, kT], writes=[ps])
                            pb = pbs.get()
                            if near:
                                tm = tms.get()
                                off = 256 - w
                                c.op("dve", lambda e: e.scalar_tensor_tensor(out=tm[:, 0:w], in0=ps[:, 0:w], scalar=scale,
                                                                             in1=nbt[:, off:off + w], op0=ALU.mult, op1=ALU.add),
                                     reads=[ps, nbt], writes=[tm])
                                c.op("act", lambda e: e.activation(out=pb[:, 0:w], in_=tm[:, 0:w], func=AF.Exp),
                                     reads=[tm], writes=[pb])
                            else:
                                c.op("act", lambda e: e.activation(out=pb[:, 0:w], in_=ps[:, 0:w], func=AF.Exp, scale=scale,
                                                                   bias=c31[:, hb:hb + 1]), reads=[ps, c31], writes=[pb])
                            if use_mask:
                                c.op("pool", lambda e: e.tensor_tensor(out=pb[:, 0:w], in0=pb[:, 0:w], in1=mk[:, j0:j0 + w], op=ALU.mult),
                                     reads=[pb, mk], writes=[pb])
                            pT = pTp.get()
                            nb_ = w // 128
                            for jb in range(nb_):
                                c.op("pe", lambda e: e.transpose(pT[:, jb, :], pb[:, jb * 128:(jb + 1) * 128], ident_b[:]),
                                     reads=[pb, ident_b], writes=[pT])
                            pS = pTs.get()
                            c.op("dve", lambda e: e.tensor_copy(out=pS[:, 0:nb_, :], in_=pT[:, 0:nb_, :]), reads=[pT], writes=[pS])
                            for jb in range(nb_):
                                c.op("pe", lambda e: e.matmul(po[:, 0:129], pS[:, jb, :], vx[:, j0 // 128 + jb, :],
                                                              start=(bi == 0), stop=(bi == nblk - 1)),
                                     reads=[pS, vx], writes=[po])
                                bi += 1
                    post(es2, tb, pol, ptr)
                c.barrier()

        def mix_dsa(l):
            with ExitStack() as es2:
                ik2 = c.sb([128, S], BF16, "ik2", es2)
                sc = c.sb([128, S], F32, "sc", es2)
                mkr = Rot([c.sb([128, S], BF16, "mkr%d" % i, es2) for i in range(2)])
                iqs = Rot([c.sb([128, 4, 128], BF16, "iq%d" % i, es2) for i in range(2)])
                iws = Rot([c.sb([128, 8], F32, "iw%d" % i, es2) for i in range(2)])
                aiws = Rot([c.sb([128, 8], F32, "aiw%d" % i, es2) for i in range(2)])
                sgns = Rot([c.sb([128, 8], F32, "sgn%d" % i, es2) for i in range(2)])
                rts = Rot([c.sb([128, 512], F32, "rt%d" % i, es2) for i in range(3)])
                sm = c.sb([128, 16], F32, "sm", es2)
                dtab = c.sb([128, NITER], F32, "dtab", es2)
                dgt = c.sb([128, 128], F32, "dgt", es2)
                pss = Rot([c.ps([128, 512], F32, "psI%d" % i, es2) for i in range(6)])
                c.dma("sp", ik2[0:64, :], hT[O_IK:O_IK + 64, :], reads=[hT], writes=[ik2])
                c.dma("sp", ik2[64:128, :], hT[O_IK:O_IK + 64, :], reads=[hT], writes=[ik2])
                for tb in range(NB):
                    L = (tb + 1) * 128
                    iq = iqs.get(); iw = iws.get(); aiw = aiws.get(); sgn = sgns.get()
                    c.dma("sp", iq[:], hT[O_IQ:O_IQ + 512, tb * 128:(tb + 1) * 128].rearrange("(pr p) t -> p pr t", p=128),
                          reads=[hT], writes=[iq])
                    c.dma("sp", iw[:], iwt[tb * 128:(tb + 1) * 128, :], reads=[iwt], writes=[iw])
                    c.op("act", lambda e: e.activation(out=aiw[:], in_=iw[:], func=AF.Abs), reads=[iw], writes=[aiw])
                    c.op("act", lambda e: e.activation(out=sgn[:], in_=iw[:], func=AF.Sign), reads=[iw], writes=[sgn])
                    for j0 in range(0, L, 512):
                        w = min(512, L - j0)
                        for hh in range(8):
                            pr, base = hh // 2, (hh % 2) * 64
                            ps = pss.get()
                            c.op("pe", lambda e: e.matmul(ps[:, 0:w], iq[base:base + 64, pr, :], ik2[base:base + 64, j0:j0 + w],
                                                          start=True, stop=True), reads=[iq, ik2], writes=[ps])
                            rt = rts.get()
                            c.op("act", lambda e: e.activation(out=rt[:, 0:w], in_=ps[:, 0:w], func=AF.Relu,
                                                               scale=aiw[:, hh:hh + 1]), reads=[ps, aiw], writes=[rt])
                            if hh == 0:
                                c.op("dve", lambda e: e.tensor_scalar(out=sc[:, j0:j0 + w], in0=rt[:, 0:w], scalar1=sgn[:, 0:1],
                                                                      scalar2=None, op0=ALU.mult), reads=[rt, sgn], writes=[sc])
                            else:
                                c.op("dve", lambda e: e.scalar_tensor_tensor(out=sc[:, j0:j0 + w], in0=rt[:, 0:w],
                                                                             scalar=sgn[:, hh:hh + 1], in1=sc[:, j0:j0 + w],
                                                                             op0=ALU.mult, op1=ALU.add),
                                     reads=[rt, sgn, sc], writes=[sc])
                    c.op("dve", lambda e: e.tensor_tensor(out=sc[:, L - 128:L], in0=sc[:, L - 128:L], in1=cneg[:], op=ALU.add),
                         reads=[sc, cneg], writes=[sc])
                    c.op("dve", lambda e: e.tensor_reduce(out=sm[:, 0:1], in_=sc[:, 0:L], axis=AX.X, op=ALU.max),
                         reads=[sc], writes=[sm])
                    c.op("dve", lambda e: e.tensor_tensor(out=dgt[:], in0=sc[:, L - 128:L], in1=cpos[:], op=ALU.add),
                         reads=[sc, cpos], writes=[dgt])
                    c.op("dve", lambda e: e.tensor_reduce(out=sm[:, 1:2], in_=dgt[:], axis=AX.X, op=ALU.min),
                         reads=[dgt], writes=[sm])
                    if tb > 0:
                        c.op("dve", lambda e: e.tensor_reduce(out=sm[:, 2:3], in_=sc[:, 0:L - 128], axis=AX.X, op=ALU.min),
                             reads=[sc], writes=[sm])
                        c.op("dve", lambda e: e.tensor_tensor(out=sm[:, 1:2], in0=sm[:, 1:2], in1=sm[:, 2:3], op=ALU.min),
                             reads=[sm], writes=[sm])
                    c.op("dve", lambda e: e.tensor_tensor(out=sm[:, 3:4], in0=sm[:, 0:1], in1=sm[:, 1:2], op=ALU.subtract),
                         reads=[sm], writes=[sm])
                    c.op("dve", lambda e: e.tensor_scalar(out=dtab[:], in0=p2tab[:], scalar1=sm[:, 3:4], scalar2=None, op0=ALU.mult),
                         reads=[sm, p2tab], writes=[dtab])
                    mk = mkr.get()
                    for k in range(NITER):
                        c.op("dve", lambda e: e.tensor_tensor(out=sm[:, 4:5], in0=sm[:, 1:2], in1=dtab[:, k:k + 1], op=ALU.add),
                             reads=[sm, dtab], writes=[sm])
                        c.op("dve", lambda e: e.tensor_scalar(out=mk[:, 0:L], in0=sc[:, 0:L], scalar1=sm[:, 4:5], scalar2=0.0,
                                                              op0=ALU.is_ge, op1=ALU.add, accum_out=sm[:, 5:6]),
                             reads=[sc, sm], writes=[mk, sm])
                        c.op("dve", lambda e: e.tensor_scalar(out=sm[:, 6:7], in0=sm[:, 5:6], scalar1=TOPK - 0.5,
                                                              scalar2=dtab[:, k:k + 1], op0=ALU.is_ge, op1=ALU.mult),
                             reads=[sm, dtab], writes=[sm])
                        c.op("dve", lambda e: e.tensor_tensor(out=sm[:, 1:2], in0=sm[:, 1:2], in1=sm[:, 6:7], op=ALU.add),
                             reads=[sm], writes=[sm])
                    c.op("dve", lambda e: e.tensor_scalar(out=mk[:, 0:L], in0=sc[:, 0:L], scalar1=sm[:, 1:2], scalar2=None,
                                                          op0=ALU.is_ge), reads=[sc, sm], writes=[mk])
                    md, r0 = mrows(tb)
                    c.dma("sp", md[r0:r0 + 128, 0:L], mk[:, 0:L], reads=[mk], writes=[md])
                c.barrier()
            for h in range(4):
                def post(es2, tb, pol, ptr, h=h):
                    po = pol[0]
                    rl = small.get()
                    c.op("dve", lambda e: e.reciprocal(out=rl[:, 0:1], in_=po[:, 128:129]), reads=[po], writes=[rl])
                    ob = obuf.get()
                    c.op("act", lambda e: e.activation(out=ob[:], in_=po[:, 0:128], func=AF.Copy, scale=rl[:, 0:1]),
                         reads=[po, rl], writes=[ob])
                    c.op("pe", lambda e: e.transpose(ptr[:, 0, :], ob[:], ident_b[:]), reads=[ob, ident_b], writes=[ptr])
                    ot = obuf.get()
                    c.op("dve", lambda e: e.tensor_copy(out=ot[:], in_=ptr[:, 0, :]), reads=[ptr], writes=[ot])
                    c.dma("sp", yT[h * 128:(h + 1) * 128, tb * 128:(tb + 1) * 128], ot[:], reads=[ot], writes=[yT])
                attention_pass(l, O_AQ + h * 128, O_AK + h * 128, 128, 1, h * 128, 128 ** -0.5, h, True, post)

        def mix_diff(l):
            with ExitStack() as es3:
                dl = c.sb([128, 4, 64], F32, "dl", es3)
                lam = c.sb([128, 8], F32, "lam", es3)
                sg = c.sb([128, 128], F32, "sgt", es3)
                ofs = Rot([c.sb([128, 128], F32, "of%d" % i, es3) for i in range(4)])
                c.dma("sp", dl[:], dlam[l].rearrange("p (a b) -> p a b", a=4), writes=[dl])
                c.dma("sp", sg[:], sublng[l], writes=[sg])
                t0 = ofs.get()
                c.op("dve", lambda e: e.tensor_tensor(out=t0[:, 0:64], in0=dl[:, 0, :], in1=dl[:, 1, :], op=ALU.mult), reads=[dl], writes=[t0])
                c.op("dve", lambda e: e.tensor_reduce(out=lam[:, 0:1], in_=t0[:, 0:64], axis=AX.X, op=ALU.add), reads=[t0], writes=[lam])
                c.op("dve", lambda e: e.tensor_tensor(out=t0[:, 64:128], in0=dl[:, 2, :], in1=dl[:, 3, :], op=ALU.mult), reads=[dl], writes=[t0])
                c.op("dve", lambda e: e.tensor_reduce(out=lam[:, 1:2], in_=t0[:, 64:128], axis=AX.X, op=ALU.add), reads=[t0], writes=[lam])
                c.op("act", lambda e: e.activation(out=lam[:, 2:4], in_=lam[:, 0:2], func=AF.Exp), reads=[lam], writes=[lam])
                c.op("dve", lambda e: e.tensor_tensor(out=lam[:, 4:5], in0=lam[:, 3:4], in1=lam[:, 2:3], op=ALU.subtract), reads=[lam], writes=[lam])
                c.op("dve", lambda e: e.tensor_scalar(out=lam[:, 4:5], in0=lam[:, 4:5], scalar1=lamc[:, l, 0:1], scalar2=None, op0=ALU.add), reads=[lam, lamc], writes=[lam])
                for h in range(4):
                    def post(es2, tb, pol, ptr, h=h):
                        p1, p2 = pol
                        rl = small.get()
                        c.op("dve", lambda e: e.reciprocal(out=rl[:, 0:1], in_=p1[:, 128:129]), reads=[p1], writes=[rl])
                        c.op("dve", lambda e: e.reciprocal(out=rl[:, 1:2], in_=p2[:, 128:129]), reads=[p2], writes=[rl])
                        c.op("dve", lambda e: e.tensor_tensor(out=rl[:, 1:2], in0=rl[:, 1:2], in1=lam[:, 4:5], op=ALU.mult), reads=[rl, lam], writes=[rl])
                        o1 = ofs.get()
                        c.op("act", lambda e: e.activation(out=o1[:], in_=p1[:, 0:128], func=AF.Copy, scale=rl[:, 0:1]),
                             reads=[p1, rl], writes=[o1])
                        o = ofs.get()
                        c.op("dve", lambda e: e.scalar_tensor_tensor(out=o[:], in0=p2[:, 0:128], scalar=rl[:, 1:2], in1=o1[:],
                                                                     op0=ALU.mult, op1=ALU.add), reads=[p2, rl, o1], writes=[o])
                        c.op("act", lambda e: e.activation(out=o1[:], in_=o[:], func=AF.Square, accum_out=rl[:, 2:3]),
                             reads=[o], writes=[o1, rl])
                        c.op("act", lambda e: e.activation(out=rl[:, 3:4], in_=rl[:, 2:3], func=AF.Sqrt, scale=1.0 / 128, bias=EPS),
                             reads=[rl], writes=[rl])
                        c.op("dve", lambda e: e.reciprocal(out=rl[:, 3:4], in_=rl[:, 3:4]), reads=[rl], writes=[rl])
                        ob = obuf.get()
                        c.op("dve", lambda e: e.scalar_tensor_tensor(out=ob[:], in0=o[:], scalar=rl[:, 3:4], in1=sg[:],
                                                                     op0=ALU.mult, op1=ALU.mult), reads=[o, rl, sg], writes=[ob])
                        c.op("pe", lambda e: e.transpose(ptr[:, 0, :], ob[:], ident_b[:]), reads=[ob, ident_b], writes=[ptr])
                        ot = obuf.get()
                        c.op("act", lambda e: e.activation(out=ot[:], in_=ptr[:, 0, :], func=AF.Copy, scale=lamc[:, l, 1:2]), reads=[ptr, lamc], writes=[ot])
                        c.dma("sp", yT[1024 + h * 128:1024 + (h + 1) * 128, tb * 128:(tb + 1) * 128], ot[:], reads=[ot], writes=[yT])
                    attention_pass(l, O_CQ + h * 128, O_CK + h * 128, 64, 2, 512 + h * 128, 64 ** -0.5, 4 + h, False, post)
                c.barrier()

        def mix_s5(l):
            TWO_PI = 2.0 * math.pi
            MAGIC = 12582912.0
            PI_LO = 3.1415925
            with ExitStack() as es2:
                pr = c.sb([128, 24, 16], F32, "s5pr", es2)
                one16 = c.sb([128, 512], F32, "one16", es2)
                iot = c.sb([128, 512], F32, "iot", es2)
                bpad = c.sb([128, 32, 128], BF16, "bpad", es2)
                cpad = c.sb([128, 32, 128], BF16, "cpad", es2)
                dsk = c.sb([128, 4], F32, "dsk", es2)
                zst = c.sb([128, 16, 2], F32, "zst", es2)
                c.op("dve", lambda e: e.memset(one16[:], 1.0), writes=[one16])
                c.op("dve", lambda e: e.memset(zst[:], 0.0), writes=[zst])
                c.dma("sp", iot[:], iota_in, writes=[iot])
                c.dma("sp", pr[:, 0, :], s5ar[l], writes=[pr])
                c.dma("sp", pr[:, 1, :], s5ai[l], writes=[pr])
                c.dma("sp", pr[:, 2, :], s5ldt[l], writes=[pr])
                c.dma("sp", dsk[:], s5d[l], writes=[dsk])
                c.dma("pool", bpad[:], s5b[l].rearrange("k c s -> c k s"), writes=[bpad])
                c.dma("pool", cpad[:], s5c[l].rearrange("k s c -> s k c"), writes=[cpad])
                P = lambda i: pr[:, i, :]

                def tt_(o, a, b, op, eng="dve"):
                    c.op(eng, lambda e: e.tensor_tensor(out=o, in0=a, in1=b, op=op), reads=[pr], writes=[pr])

                def ts_(o, a, s1, op0, s2=None, op1=None):
                    if op1 is None:
                        c.op("dve", lambda e: e.tensor_scalar(out=o, in0=a, scalar1=s1, scalar2=None, op0=op0), reads=[pr], writes=[pr])
                    else:
                        c.op("dve", lambda e: e.tensor_scalar(out=o, in0=a, scalar1=s1, scalar2=s2, op0=op0, op1=op1), reads=[pr], writes=[pr])

                def sincos(o_sin, o_cos, turns, tmp, tmp2):
                    rw = [pr, one16]
                    for (o, shift) in ((o_sin, 0.0), (o_cos, 0.25)):
                        c.op("dve", lambda e: e.tensor_scalar(out=tmp, in0=turns, scalar1=shift, scalar2=None, op0=ALU.add), reads=rw, writes=rw)
                        c.op("dve", lambda e: e.tensor_scalar(out=tmp2, in0=tmp, scalar1=MAGIC, scalar2=None, op0=ALU.add), reads=rw, writes=rw)
                        c.op("dve", lambda e: e.tensor_scalar(out=tmp2, in0=tmp2, scalar1=-MAGIC, scalar2=None, op0=ALU.add), reads=rw, writes=rw)
                        c.op("dve", lambda e: e.tensor_tensor(out=tmp, in0=tmp, in1=tmp2, op=ALU.subtract), reads=rw, writes=rw)
                        c.op("dve", lambda e: e.tensor_scalar(out=tmp, in0=tmp, scalar1=TWO_PI, scalar2=PI_LO, op0=ALU.mult, op1=ALU.min), reads=rw, writes=rw)
                        c.op("dve", lambda e: e.tensor_scalar(out=tmp, in0=tmp, scalar1=-PI_LO, scalar2=None, op0=ALU.max), reads=rw, writes=rw)
                        c.op("act", lambda e: e.activation(out=o, in_=tmp, func=AF.Sin), reads=rw, writes=rw)

                c.op("act", lambda e: e.activation(out=P(2), in_=P(2), func=AF.Exp), reads=[pr], writes=[pr])
                tt_(P(13), P(0), P(2), ALU.mult)
                c.op("act", lambda e: e.activation(out=P(3), in_=P(13), func=AF.Exp), reads=[pr], writes=[pr])
                tt_(P(4), P(1), P(2), ALU.mult)
                ts_(P(4), P(4), 1.0 / TWO_PI, ALU.mult)
                sincos(P(6), P(5), P(4), P(14), P(15))
                tt_(P(7), P(3), P(5), ALU.mult)
                tt_(P(8), P(3), P(6), ALU.mult)
                tt_(P(9), P(0), P(0), ALU.mult)
                tt_(P(13), P(1), P(1), ALU.mult)
                tt_(P(9), P(9), P(13), ALU.add)
                c.op("dve", lambda e: e.reciprocal(out=P(9), in_=P(9)), reads=[pr], writes=[pr])
                ts_(P(10), P(7), -1.0, ALU.add)
                tt_(P(13), P(10), P(0), ALU.mult)
                tt_(P(14), P(8), P(1), ALU.mult)
                tt_(P(11), P(13), P(14), ALU.add)
                tt_(P(11), P(11), P(9), ALU.mult)
                tt_(P(13), P(8), P(0), ALU.mult)
                tt_(P(14), P(10), P(1), ALU.mult)
                tt_(P(12), P(13), P(14), ALU.subtract)
                tt_(P(12), P(12), P(9), ALU.mult)

                tabs = c.sb([128, 4, 5, 512], F32, "s5tab", es2)
                wk = Rot([c.sb([128, 512], F32, "s5w%d" % i, es2) for i in range(8)])
                xbs = Rot([c.sb([128, 512], BF16, "s5xb%d" % i, es2) for i in range(4)])
                uts = Rot([c.sb([128, 512], BF16, "s5u%d" % i, es2) for i in range(2)])
                zbs = Rot([c.sb([128, 512], BF16, "s5z%d" % i, es2) for i in range(2)])
                sml = Rot([c.sb([128, 8], F32, "s5s%d" % i, es2) for i in range(4)])
                pps = Rot([c.ps([128, 512], F32, "s5p%d" % i, es2) for i in range(4)])
                pys = Rot([c.ps([128, 512], F32, "s5y%d" % i, es2) for i in range(2)])
                for cc in range(4):
                    for kk in range(4):
                        k = cc * 4 + kk
                        tb_ = lambda i: tabs[:, kk, i, :]
                        t1 = wk.get(); t2 = wk.get()
                        rw = [tabs, t1, t2]
                        c.op("dve", lambda e: e.tensor_scalar(out=t1[:], in0=iot[:], scalar1=pr[:, 4, k:k + 1], scalar2=None, op0=ALU.mult),
                             reads=[iot, pr], writes=[t1])
                        t3 = wk.get()
                        rw = [tabs, t1, t2, t3]
                        for (oi, shift) in ((3, 0.0), (2, 0.25)):
                            c.op("dve", lambda e: e.tensor_scalar(out=t2[:], in0=t1[:], scalar1=shift, scalar2=None, op0=ALU.add), reads=rw, writes=rw)
                            c.op("dve", lambda e: e.tensor_scalar(out=t3[:], in0=t2[:], scalar1=MAGIC, scalar2=None, op0=ALU.add), reads=rw, writes=rw)
                            c.op("dve", lambda e: e.tensor_scalar(out=t3[:], in0=t3[:], scalar1=-MAGIC, scalar2=None, op0=ALU.add), reads=rw, writes=rw)
                            c.op("dve", lambda e: e.tensor_tensor(out=t2[:], in0=t2[:], in1=t3[:], op=ALU.subtract), reads=rw, writes=rw)
                            c.op("dve", lambda e: e.tensor_scalar(out=t2[:], in0=t2[:], scalar1=TWO_PI, scalar2=PI_LO, op0=ALU.mult, op1=ALU.min), reads=rw, writes=rw)
                            c.op("dve", lambda e: e.tensor_scalar(out=t2[:], in0=t2[:], scalar1=-PI_LO, scalar2=None, op0=ALU.max), reads=rw, writes=rw)
                            c.op("act", lambda e: e.activation(out=tb_(oi), in_=t2[:], func=AF.Sin), reads=rw, writes=rw)
                        c.op("dve", lambda e: e.tensor_scalar(out=t1[:], in0=tb_(3), scalar1=pr[:, 12, k:k + 1], scalar2=None, op0=ALU.mult), reads=rw + [pr], writes=rw)
                        c.op("dve", lambda e: e.scalar_tensor_tensor(out=tb_(0), in0=tb_(2), scalar=pr[:, 11, k:k + 1], in1=t1[:], op0=ALU.mult, op1=ALU.add), reads=rw + [pr], writes=rw)
                        c.op("dve", lambda e: e.tensor_scalar(out=t1[:], in0=tb_(3), scalar1=pr[:, 11, k:k + 1], scalar2=None, op0=ALU.mult), reads=rw + [pr], writes=rw)
                        c.op("dve", lambda e: e.scalar_tensor_tensor(out=tb_(1), in0=tb_(2), scalar=pr[:, 12, k:k + 1], in1=t1[:], op0=ALU.mult, op1=ALU.subtract), reads=rw + [pr], writes=rw)
                        c.op("dve", lambda e: e.tensor_scalar(out=tb_(4), in0=one16[:], scalar1=pr[:, 3, k:k + 1], scalar2=None, op0=ALU.mult), reads=[one16, pr], writes=rw)
                    for tc in range(NTT):
                        tsl = slice(tc * 512, (tc + 1) * 512)
                        ut = uts.get()
                        c.dma("sp", ut[:], hT[O_BU + cc * 128:O_BU + (cc + 1) * 128, tsl], reads=[hT], writes=[ut])
                        py = pys.get()
                        for kk in range(4):
                            k = cc * 4 + kk
                            tb_ = lambda i: tabs[:, kk, i, :]
                            pre = pps.get(); pim = pps.get()
                            c.op("pe", lambda e: e.matmul(pre[:], bpad[:, 2 * k, :], ut[:], start=True, stop=True), reads=[bpad, ut], writes=[pre])
                            c.op("pe", lambda e: e.matmul(pim[:], bpad[:, 2 * k + 1, :], ut[:], start=True, stop=True), reads=[bpad, ut], writes=[pim])
                            a1 = wk.get(); a2 = wk.get(); bre = wk.get(); bim = wk.get()
                            c.op("dve", lambda e: e.tensor_tensor(out=a1[:], in0=tb_(0), in1=pre[:], op=ALU.mult), reads=[tabs, pre], writes=[a1])
                            c.op("dve", lambda e: e.tensor_tensor(out=a2[:], in0=tb_(1), in1=pim[:], op=ALU.mult), reads=[tabs, pim], writes=[a2])
                            c.op("pool", lambda e: e.tensor_tensor(out=bre[:], in0=a1[:], in1=a2[:], op=ALU.subtract), reads=[a1, a2], writes=[bre])
                            a3 = wk.get(); a4 = wk.get()
                            c.op("dve", lambda e: e.tensor_tensor(out=a3[:], in0=tb_(0), in1=pim[:], op=ALU.mult), reads=[tabs, pim], writes=[a3])
                            c.op("dve", lambda e: e.tensor_tensor(out=a4[:], in0=tb_(1), in1=pre[:], op=ALU.mult), reads=[tabs, pre], writes=[a4])
                            c.op("pool", lambda e: e.tensor_tensor(out=bim[:], in0=a3[:], in1=a4[:], op=ALU.add), reads=[a3, a4], writes=[bim])
                            c.op("dve", lambda e: e.tensor_tensor_scan(out=bre[:], data0=tb_(4), data1=bre[:], initial=zst[:, k, 0:1],
                                                                       op0=ALU.mult, op1=ALU.add), reads=[tabs, bre, zst], writes=[bre])
                            c.op("dve", lambda e: e.tensor_tensor_scan(out=bim[:], data0=tb_(4), data1=bim[:], initial=zst[:, k, 1:2],
                                                                       op0=ALU.mult, op1=ALU.add), reads=[tabs, bim, zst], writes=[bim])
                            c.op("dve", lambda e: e.tensor_tensor(out=a1[:], in0=tb_(2), in1=bre[:], op=ALU.mult), reads=[tabs, bre], writes=[a1])
                            c.op("pool", lambda e: e.tensor_tensor(out=a2[:], in0=tb_(3), in1=bim[:], op=ALU.mult), reads=[tabs, bim], writes=[a2])
                            c.op("dve", lambda e: e.tensor_tensor(out=a1[:], in0=a1[:], in1=a2[:], op=ALU.subtract), reads=[a1, a2], writes=[a1])
                            c.op("dve", lambda e: e.tensor_tensor(out=a3[:], in0=tb_(2), in1=bim[:], op=ALU.mult), reads=[tabs, bim], writes=[a3])
                            c.op("pool", lambda e: e.tensor_tensor(out=a4[:], in0=tb_(3), in1=bre[:], op=ALU.mult), reads=[tabs, bre], writes=[a4])
                            c.op("dve", lambda e: e.tensor_tensor(out=a3[:], in0=a3[:], in1=a4[:], op=ALU.add), reads=[a3, a4], writes=[a3])
                            xr = xbs.get(); xi = xbs.get()
                            c.op("act", lambda e: e.copy(out=xr[:], in_=a1[:]), reads=[a1], writes=[xr])
                            c.op("act", lambda e: e.activation(out=xi[:], in_=a3[:], func=AF.Copy, scale=-1.0), reads=[a3], writes=[xi])
                            sm_ = sml.get()
                            c.op("dve", lambda e: e.tensor_tensor(out=sm_[:, 0:1], in0=pr[:, 5, k:k + 1], in1=a1[:, 511:512], op=ALU.mult), reads=[pr, a1], writes=[sm_])
                            c.op("dve", lambda e: e.tensor_tensor(out=sm_[:, 1:2], in0=pr[:, 6, k:k + 1], in1=a3[:, 511:512], op=ALU.mult), reads=[pr, a3], writes=[sm_])
                            c.op("dve", lambda e: e.tensor_tensor(out=zst[:, k, 0:1], in0=sm_[:, 0:1], in1=sm_[:, 1:2], op=ALU.subtract), reads=[sm_], writes=[zst])
                            c.op("dve", lambda e: e.tensor_tensor(out=sm_[:, 2:3], in0=pr[:, 5, k:k + 1], in1=a3[:, 511:512], op=ALU.mult), reads=[pr, a3], writes=[sm_])
                            c.op("dve", lambda e: e.tensor_tensor(out=sm_[:, 3:4], in0=pr[:, 6, k:k + 1], in1=a1[:, 511:512], op=ALU.mult), reads=[pr, a1], writes=[sm_])
                            c.op("dve", lambda e: e.tensor_tensor(out=zst[:, k, 1:2], in0=sm_[:, 2:3], in1=sm_[:, 3:4], op=ALU.add), reads=[sm_], writes=[zst])
                            c.op("pe", lambda e: e.matmul(py[:], cpad[:, 2 * k, :], xr[:], start=(kk == 0), stop=False), reads=[cpad, xr], writes=[py])
                            c.op("pe", lambda e: e.matmul(py[:], cpad[:, 2 * k + 1, :], xi[:], start=False, stop=(kk == 3)), reads=[cpad, xi], writes=[py])
                        yv = wk.get(); q = wk.get()
                        c.op("dve", lambda e: e.scalar_tensor_tensor(out=yv[:], in0=ut[:], scalar=dsk[:, cc:cc + 1], in1=py[:], op0=ALU.mult, op1=ALU.add),
                             reads=[ut, dsk, py], writes=[yv])
                        c.op("act", lambda e: e.activation(out=q[:], in_=yv[:], func=AF.Square), reads=[yv], writes=[q])
                        c.op("dve", lambda e: e.tensor_scalar(out=q[:], in0=q[:], scalar1=0.044715, scalar2=1.0, op0=ALU.mult, op1=ALU.add), reads=[q], writes=[q])
                        c.op("dve", lambda e: e.tensor_tensor(out=q[:], in0=q[:], in1=yv[:], op=ALU.mult), reads=[q, yv], writes=[q])
                        c.op("act", lambda e: e.activation(out=q[:], in_=q[:], func=AF.Sigmoid, scale=2.0 * math.sqrt(2.0 / math.pi)), reads=[q], writes=[q])
                        zb = zbs.get()
                        c.op("dve", lambda e: e.tensor_tensor(out=zb[:], in0=q[:], in1=yv[:], op=ALU.mult), reads=[q, yv], writes=[zb])
                        c.dma("sp", zT[cc * 128:(cc + 1) * 128, tsl], zb[:], reads=[zb], writes=[zT])
                c.barrier()
            with ExitStack() as es2:
                wg = c.sb([128, 4, 512], BF16, "wglu", es2)
                zts = Rot([c.sb([128, 4, 512], BF16, "zt%d" % i, es2) for i in range(2)])
                sgs = Rot([c.sb([128, 512], F32, "gsg%d" % i, es2) for i in range(2)])
                obs = Rot([c.sb([128, 512], BF16, "gob%d" % i, es2) for i in range(3)])
                pg = Rot([c.ps([128, 512], F32, "gps%d" % i, es2) for i in range(3)])
                c.dma("pool", wg[:], s5wglu[l].rearrange("(kc p) n -> p kc n", p=128), writes=[wg])
                for tc in range(NTT):
                    tsl = slice(tc * 512, (tc + 1) * 512)
                    zt = zts.get()
                    c.dma("sp", zt[:], zT[:, tsl].rearrange("(kc p) t -> p kc t", p=128), reads=[zT], writes=[zt])
                    for oc in range(4):
                        ps = pg.get()
                        for kc in range(4):
                            c.op("pe", lambda e: e.matmul(ps[:], wg[:, kc, oc * 128:(oc + 1) * 128], zt[:, kc, :], start=(kc == 0), stop=(kc == 3)),
                                 reads=[wg, zt], writes=[ps])
                        sg = sgs.get()
                        c.op("act", lambda e: e.activation(out=sg[:], in_=ps[:], func=AF.Sigmoid), reads=[ps], writes=[sg])
                        ob = obs.get()
                        c.op("dve", lambda e: e.tensor_tensor(out=ob[:], in0=zt[:, oc, :], in1=sg[:], op=ALU.mult), reads=[zt, sg], writes=[ob])
                        c.dma("sp", yT[512 + oc * 128:512 + (oc + 1) * 128, tsl], ob[:], reads=[ob], writes=[yT])
                c.barrier()

        def mix_mlstm(l):
            with ExitStack() as es2:
                cw = c.sb([128, 8, 4], F32, "cw", es2)
                raws = Rot([c.sb([128, 515], BF16, "raw%d" % i, es2) for i in range(3)])
                accs = Rot([c.sb([128, 512], F32, "cacc%d" % i, es2) for i in range(3)])
                outs = Rot([c.sb([128, 512], BF16, "cout%d" % i, es2) for i in range(3)])
                c.dma("sp", cw[:], convw[l], writes=[cw])
                for ch in range(8):
                    r0 = O_DQK + ch * 128
                    for tc in range(NTT):
                        t0 = tc * 512
                        raw = raws.get()
                        if tc == 0:
                            c.op("pool", lambda e: e.memset(raw[:, 0:3], 0.0), writes=[raw])
                            c.dma("sp", raw[:, 3:515], hT[r0:r0 + 128, 0:512], reads=[hT], writes=[raw])
                        else:
                            c.dma("sp", raw[:], hT[r0:r0 + 128, t0 - 3:t0 + 512], reads=[hT], writes=[raw])
                        acc = accs.get()
                        c.op("dve", lambda e: e.tensor_scalar(out=acc[:], in0=raw[:, 0:512], scalar1=cw[:, ch, 0:1], scalar2=None, op0=ALU.mult),
                             reads=[raw, cw], writes=[acc])
                        for j in range(1, 4):
                            c.op("dve", lambda e: e.scalar_tensor_tensor(out=acc[:], in0=raw[:, j:j + 512], scalar=cw[:, ch, j:j + 1], in1=acc[:],
                                                                         op0=ALU.mult, op1=ALU.add), reads=[raw, cw, acc], writes=[acc])
                        ob = outs.get()
                        if ch < 4:
                            c.op("act", lambda e: e.activation(out=ob[:], in_=acc[:], func=AF.Silu), reads=[acc], writes=[ob])
                        else:
                            c.op("act", lambda e: e.activation(out=acc[:], in_=acc[:], func=AF.Silu), reads=[acc], writes=[acc])
                            c.op("dve", lambda e: e.tensor_scalar(out=ob[:], in0=acc[:], scalar1=128 ** -0.5, scalar2=None, op0=ALU.mult),
                                 reads=[acc], writes=[ob])
                        c.dma("sp", qkc[ch * 128:(ch + 1) * 128, t0:t0 + 512], ob[:], reads=[ob], writes=[qkc])
                c.barrier()
            NEG = -3.0e38
            for h in range(4):
                with ExitStack() as es2:
                    kT = c.sb([128, S], BF16, "mkT", es2)
                    qT = c.sb([128, S], BF16, "mqT", es2)
                    doT = c.sb([128, S], BF16, "mdoT", es2)
                    vx = c.sb([128, NB, 129], BF16, "mvx", es2)
                    gb = c.sb([128, 8], F32, "mgb", es2)
                    ga = c.sb([128, 8, 128], F32, "mga", es2)
                    g3 = c.sb([128, 3, 128], F32, "mg3", es2)
                    col = c.sb([128, 8], F32, "mcol", es2)
                    rows = c.sb([1, 8, 128], F32, "mrows", es2)
                    abrep = c.sb([128, 2, 128], F32, "mab", es2)
                    wT = c.sb([128, 128], F32, "mwT", es2)
                    rT = c.sb([128, 128], F32, "mrT", es2)
                    cn = c.sb([128, 129], F32, "mcn", es2)
                    cbf = c.sb([128, 128], BF16, "mcbf", es2)
                    nrep = c.sb([128, 128], BF16, "mnrep", es2)
                    ones_b = c.sb([128, 128], BF16, "mones", es2)
                    one_r = c.sb([128, 128], F32, "moner", es2)
                    wk = Rot([c.sb([128, 128], F32, "mw%d" % i, es2) for i in range(6)])
                    wb = Rot([c.sb([128, 129], BF16, "mb%d" % i, es2) for i in range(6)])
                    pbc = c.ps([128, 512], F32, "mpbc", es2)
                    pkq = c.ps([128, 512], F32, "mpkq", es2)
                    pnum = c.ps([128, 512], F32, "mpnum", es2)
                    pden = c.ps([128, 512], F32, "mpden", es2)
                    pcl = c.ps([128, 512], F32, "mpcl", es2)
                    pkt = c.ps([128, 8, 128], BF16, "mpkt", es2)
                    ptr2 = c.ps([128, 512], F32, "mptr", es2)
                    c.dma("sp", qT[:], qkc[h * 128:(h + 1) * 128, :], reads=[qkc], writes=[qT])
                    c.dma("sp", kT[:], qkc[512 + h * 128:512 + (h + 1) * 128, :], reads=[qkc], writes=[kT])
                    c.dma("sp", doT[:], hT[O_DO + h * 128:O_DO + (h + 1) * 128, :], reads=[hT], writes=[doT])
                    c.op("pool", lambda e: e.memset(vx[:], 1.0), writes=[vx])
                    c.dma("sp", vx[:, :, 0:128], vtm[:, 1024 + h * 128:1024 + (h + 1) * 128].rearrange("(nb p) d -> p nb d", p=128),
                          reads=[vtm], writes=[vx])
                    c.dma("sp", gb[:], mbias[l], writes=[gb])
                    c.dma("sp", ga[0:NB, 0, :], giT[h, :].rearrange("(b j) -> b j", j=128), reads=[giT], writes=[ga])
                    c.dma("sp", ga[0:NB, 1, :], giT[4 + h, :].rearrange("(b j) -> b j", j=128), reads=[giT], writes=[ga])
                    c.op("dve", lambda e: e.memset(cn[:], 0.0), writes=[cn])
                    c.op("dve", lambda e: e.memset(cbf[:], 0.0), writes=[cbf])
                    c.op("dve", lambda e: e.memset(nrep[:], 0.0), writes=[nrep])
                    c.op("dve", lambda e: e.memset(ones_b[:], 1.0), writes=[ones_b])
                    c.op("dve", lambda e: e.memset(one_r[:], 1.0), writes=[one_r])
                    c.op("dve", lambda e: e.memset(rows[:], 0.0), writes=[rows])
                    G = lambda i: ga[0:NB, i, :]
                    rwg = [ga, col, gb]

                    def gop(eng, fn):
                        c.op(eng, fn, reads=rwg, writes=rwg)
                    gop("dve", lambda e: e.tensor_scalar(out=col[:, 0:1], in0=gb[:, 4 + h:5 + h], scalar1=-1.0, scalar2=None, op0=ALU.mult))
                    gop("act", lambda e: e.activation(out=G(1), in_=G(1), func=AF.Exp, scale=-1.0, bias=col[0:NB, 0:1]))
                    gop("act", lambda e: e.activation(out=G(1), in_=G(1), func=AF.Ln, bias=1.0))
                    gop("dve", lambda e: e.tensor_tensor_scan(out=G(2), data0=one_r[0:NB, :], data1=G(1), initial=0.0, op0=ALU.mult, op1=ALU.add))
                    gop("dve", lambda e: e.tensor_scalar(out=G(0), in0=G(0), scalar1=gb[0:NB, h:h + 1], scalar2=None, op0=ALU.add))
                    gop("dve", lambda e: e.tensor_tensor(out=G(3), in0=G(0), in1=G(2), op=ALU.add))
                    gop("dve", lambda e: e.tensor_tensor_scan(out=G(4), data0=G(3), data1=G(3), initial=NEG, op0=ALU.max, op1=ALU.max))
                    gop("dve", lambda e: e.tensor_scalar(out=col[0:NB, 1:2], in0=ga[0:NB, 2, 127:128], scalar1=-1.0, scalar2=None, op0=ALU.mult))
                    gop("dve", lambda e: e.tensor_tensor(out=col[0:NB, 2:3], in0=col[0:NB, 1:2], in1=ga[0:NB, 4, 127:128], op=ALU.add))
                    gop("dve", lambda e: e.tensor_scalar(out=col[0:NB, 3:4], in0=ga[0:NB, 4, 127:128], scalar1=-1.0, scalar2=None, op0=ALU.mult))
                    for (ci, ri) in ((1, 0), (2, 1)):
                        c.op("pe", lambda e: e.transpose(ptr2[0:1, 0:NB], col[0:NB, ci:ci + 1], ident_f[0:NB, 0:NB]), reads=[col, ident_f], writes=[ptr2])
                        c.op("dve", lambda e: e.tensor_copy(out=rows[0:1, ri, 0:NB], in_=ptr2[0:1, 0:NB]), reads=[ptr2], writes=[rows])
                    rw = [rows]
                    c.op("dve", lambda e: e.tensor_tensor_scan(out=rows[0:1, 2, 0:NB], data0=rows[0:1, 0, 0:NB], data1=rows[0:1, 1, 0:NB],
                                                               initial=0.0, op0=ALU.add, op1=ALU.max), reads=rw, writes=rw)
                    if NB > 1:
                        c.op("dve", lambda e: e.tensor_copy(out=rows[0:1, 3, 1:NB], in_=rows[0:1, 2, 0:NB - 1]), reads=rw, writes=rw)
                    c.op("dve", lambda e: e.tensor_tensor(out=rows[0:1, 4, 0:NB], in0=rows[0:1, 0, 0:NB], in1=rows[0:1, 3, 0:NB], op=ALU.add), reads=rw, writes=rw)
                    c.op("dve", lambda e: e.tensor_tensor(out=rows[0:1, 4, 0:NB], in0=rows[0:1, 4, 0:NB], in1=rows[0:1, 2, 0:NB], op=ALU.subtract), reads=rw, writes=rw)
                    c.op("dve", lambda e: e.tensor_tensor(out=rows[0:1, 5, 0:NB], in0=rows[0:1, 1, 0:NB], in1=rows[0:1, 2, 0:NB], op=ALU.subtract), reads=rw, writes=rw)
                    c.op("act", lambda e: e.activation(out=rows[0:1, 4:6, :], in_=rows[0:1, 4:6, :], func=AF.Exp), reads=rw, writes=rw)
                    c.op("pe", lambda e: e.matmul(pbc[:, 0:256], ones_f[0:1, :], rows[0:1, 4:6, :], start=True, stop=True), reads=[ones_f, rows], writes=[pbc])
                    c.op("dve", lambda e: e.tensor_copy(out=abrep[:], in_=pbc[:, 0:256]), reads=[pbc], writes=[abrep])
                    c.op("pe", lambda e: e.transpose(ptr2[0:NB, 0:1], rows[0:1, 3, 0:NB], ident_f[0:1, 0:1]), reads=[rows, ident_f], writes=[ptr2])
                    gop("dve", lambda e: e.tensor_copy(out=col[0:NB, 4:5], in_=ptr2[0:NB, 0:1]))
                    rwg2 = [ga, col, g3, ptr2]
                    c.op("dve", lambda e: e.tensor_scalar(out=g3[0:NB, 0, :], in0=G(4), scalar1=col[0:NB, 4:5], scalar2=None, op0=ALU.max), reads=rwg2, writes=rwg2)
                    c.op("dve", lambda e: e.tensor_tensor(out=G(5), in0=g3[0:NB, 0, :], in1=G(2), op=ALU.subtract), reads=rwg2, writes=rwg2)
                    c.op("act", lambda e: e.activation(out=g3[0:NB, 1, :], in_=g3[0:NB, 0, :], func=AF.Exp, scale=-1.0, bias=col[0:NB, 4:5]), reads=rwg2, writes=rwg2)
                    c.op("act", lambda e: e.activation(out=g3[0:NB, 2, :], in_=G(5), func=AF.Exp, scale=-1.0), reads=rwg2, writes=rwg2)
                    c.op("act", lambda e: e.activation(out=G(6), in_=G(3), func=AF.Exp, bias=col[0:NB, 3:4]), reads=rwg2, writes=rwg2)
                    c.op("pe", lambda e: e.transpose(ptr2[:, 0:NB], G(6), ident_f[0:NB, 0:NB]), reads=[ga, ident_f], writes=[ptr2])
                    c.op("dve", lambda e: e.tensor_copy(out=wT[:, 0:NB], in_=ptr2[:, 0:NB]), reads=[ptr2], writes=[wT])
                    c.op("pe", lambda e: e.transpose(ptr2[:, 0:NB], G(3), ident_f[0:NB, 0:NB]), reads=[ga, ident_f], writes=[ptr2])
                    c.op("dve", lambda e: e.tensor_copy(out=rT[:, 0:NB], in_=ptr2[:, 0:NB]), reads=[ptr2], writes=[rT])
                    for blk in range(NB):
                        bs = slice(blk * 128, (blk + 1) * 128)
                        c.op("pe", lambda e: e.matmul(pbc[:, 0:384], ident_f[0:NB, blk:blk + 1].to_broadcast([NB, 128]),
                                                      g3[0:NB, :, :], start=True, stop=True), reads=[ident_f, g3], writes=[pbc])
                        da = wk.get()
                        c.op("dve", lambda e: e.tensor_scalar(out=da[:], in0=pbc[:, 0:128], scalar1=rT[:, blk:blk + 1], scalar2=0.0,
                                                              op0=ALU.subtract, op1=ALU.max), reads=[pbc, rT], writes=[da])
                        c.op("act", lambda e: e.activation(out=da[:], in_=da[:], func=AF.Exp, scale=-1.0), reads=[da], writes=[da])
                        c.op("pool", lambda e: e.tensor_tensor(out=da[:], in0=da[:], in1=ctri[:], op=ALU.mult), reads=[da, ctri], writes=[da])
                        c.op("pe", lambda e: e.matmul(pkq[:, 0:128], kT[:, bs], qT[:, bs], start=True, stop=True), reads=[kT, qT], writes=[pkq])
                        dq_ = wb.get()
                        c.op("dve", lambda e: e.tensor_tensor(out=dq_[:, 0:128], in0=pkq[:, 0:128], in1=da[:], op=ALU.mult), reads=[pkq, da], writes=[dq_])
                        qi = wb.get()
                        c.op("dve", lambda e: e.tensor_tensor(out=qi[:, 0:128], in0=qT[:, bs], in1=pbc[:, 128:256], op=ALU.mult), reads=[qT, pbc], writes=[qi])
                        c.op("pe", lambda e: e.matmul(pnum[:, 0:128], vx[:, blk, 0:128], dq_[:, 0:128], start=True, stop=False), reads=[vx, dq_], writes=[pnum])
                        c.op("pe", lambda e: e.matmul(pnum[:, 0:128], cbf[:], qi[:, 0:128], start=False, stop=True), reads=[cbf, qi], writes=[pnum])
                        c.op("pe", lambda e: e.matmul(pden[:, 0:128], ones_b[:], dq_[:, 0:128], start=True, stop=False), reads=[ones_b, dq_], writes=[pden])
                        c.op("pe", lambda e: e.matmul(pden[:, 0:128], nrep[:], qi[:, 0:128], start=False, stop=True), reads=[nrep, qi], writes=[pden])
                        dd = wk.get()
                        c.op("act", lambda e: e.activation(out=dd[:], in_=pden[:, 0:128], func=AF.Abs), reads=[pden], writes=[dd])
                        c.op("dve", lambda e: e.tensor_tensor(out=dd[:], in0=dd[:], in1=pbc[:, 256:384], op=ALU.max), reads=[dd, pbc], writes=[dd])
                        c.op("dve", lambda e: e.reciprocal(out=dd[:], in_=dd[:]), reads=[dd], writes=[dd])
                        hd = wk.get()
                        c.op("dve", lambda e: e.tensor_tensor(out=hd[:], in0=pnum[:, 0:128], in1=dd[:], op=ALU.mult), reads=[pnum, dd], writes=[hd])
                        so = wk.get()
                        c.op("act", lambda e: e.activation(out=so[:], in_=doT[:, bs], func=AF.Sigmoid), reads=[doT], writes=[so])
                        yb = wb.get()
                        c.op("pool", lambda e: e.tensor_tensor(out=yb[:, 0:128], in0=hd[:], in1=so[:], op=ALU.mult), reads=[hd, so], writes=[yb])
                        c.dma("sp", yT[1536 + h * 128:1536 + (h + 1) * 128, bs], yb[:, 0:128], reads=[yb], writes=[yT])
                        c.op("pe", lambda e: e.transpose(pkt[:, 0, :], kT[:, bs], ident_b[:]), reads=[kT, ident_b], writes=[pkt])
                        kt = wb.get()
                        c.op("act", lambda e: e.copy(out=kt[:, 0:128], in_=pkt[:, 0, :]), reads=[pkt], writes=[kt])
                        vw = wb.get()
                        c.op("dve", lambda e: e.tensor_scalar(out=vw[:], in0=vx[:, blk, :], scalar1=wT[:, blk:blk + 1], scalar2=None, op0=ALU.mult),
                             reads=[vx, wT], writes=[vw])
                        c.op("pe", lambda e: e.matmul(pcl[:, 0:129], kt[:, 0:128], vw[:], start=True, stop=True), reads=[kt, vw], writes=[pcl])
                        t_ = wk.get()
                        c.op("dve", lambda e: e.tensor_scalar(out=t_[:, 0:128], in0=pcl[:, 0:128], scalar1=abrep[:, 1, blk:blk + 1], scalar2=None, op0=ALU.mult),
                             reads=[pcl, abrep], writes=[t_])
                        c.op("dve", lambda e: e.tensor_scalar(out=col[:, 6:7], in0=pcl[:, 128:129], scalar1=abrep[:, 1, blk:blk + 1], scalar2=None, op0=ALU.mult),
                             reads=[pcl, abrep], writes=[col])
                        c.op("dve", lambda e: e.scalar_tensor_tensor(out=cn[:, 0:128], in0=cn[:, 0:128], scalar=abrep[:, 0, blk:blk + 1], in1=t_[:, 0:128],
                                                                     op0=ALU.mult, op1=ALU.add), reads=[cn, abrep, t_], writes=[cn])
                        c.op("dve", lambda e: e.scalar_tensor_tensor(out=cn[:, 128:129], in0=cn[:, 128:129], scalar=abrep[:, 0, blk:blk + 1], in1=col[:, 6:7],
                                                                     op0=ALU.mult, op1=ALU.add), reads=[cn, abrep, col], writes=[cn])
                        c.op("act", lambda e: e.copy(out=cbf[:], in_=cn[:, 0:128]), reads=[cn], writes=[cbf])
                        c.op("pool", lambda e: e.tensor_copy(out=nrep[:], in_=cn[:, 128:129].to_broadcast([128, 128])), reads=[cn], writes=[nrep])
                    c.barrier()

        def zero_y(r0, r1):
            with ExitStack() as es2:
                z = c.sb([128, 512], BF16, "zz", es2)
                c.op("dve", lambda e: e.memset(z[:], 0.0), writes=[z])
                for kc in range(r0 // 128, r1 // 128):
                    for tt in range(NTT):
                        c.dma("sp", yT[kc * 128:(kc + 1) * 128, tt * 512:(tt + 1) * 512], z[:], reads=[z], writes=[yT])
                c.barrier()

        def mixers(l):
            if "a" in MIX:
                mix_dsa(l)
            else:
                zero_y(0, 512)
            if "b" in MIX:
                mix_s5(l)
            else:
                zero_y(512, 1024)
            if "c" in MIX:
                mix_diff(l)
            else:
                zero_y(1024, 1536)
            if "d" in MIX:
                mix_mlstm(l)
            else:
                zero_y(1536, 2048)
            if dbg is not None and l == 0:
                for _kc in range(16):
                    for _tt in range(NTT):
                        c.dma("sp", dbg_y[_kc * 128:(_kc + 1) * 128, _tt * 512:(_tt + 1) * 512],
                              yT[_kc * 128:(_kc + 1) * 128, _tt * 512:(_tt + 1) * 512], reads=[yT], writes=[Buf(None)])
                c.barrier()

        import os as _os
        for _i in range(int(_os.environ.get("EXTRA_256MB", "0"))):
            _t = c.dram([16384, 8192], BF16, "extra%d" % _i)
            c.dma("sp", _t[0:128, 0:128], ident_b[:], reads=[ident_b], writes=[_t])
        for stage in range(DEPTH + 1):
            token_phase(stage)
            if stage < DEPTH:
                mixers(stage)
        c.finish()
        print("instructions:", c.nins, {k: v for k, v in c.cnt.items() if not k.startswith("d")})
    return nc


def _col(v):
    return np.ascontiguousarray(np.asarray(v, np.float32).reshape(16, 128).T)


def prep_inputs(inp, S, DEPTH, b, xT=None, l0=0):
    f = lambda a: np.ascontiguousarray(np.asarray(a, dtype=np.float32))
    w_in = f(inp["w_in"])
    o = np.cumsum([0, 512, 512, 512, 512, 64, 8, 512, 512, 512, 512, 1024, 512, 512, 4, 4])
    (aq, ak, av, aiq, aik, aiw, bu, cq, ck, cv, dqk, dv, do, di, df) = [slice(o[i], o[i + 1]) for i in range(15)]
    wfm = np.zeros((DEPTH, D, NF), np.float32)
    wfm[:, :, O_AQ:O_AQ + 512] = w_in[:, :, aq]
    wfm[:, :, O_AK:O_AK + 512] = w_in[:, :, ak]
    wfm[:, :, O_IQ:O_IQ + 512] = w_in[:, :, aiq]
    wfm[:, :, O_IK:O_IK + 64] = w_in[:, :, aik]
    wfm[:, :, O_BU:O_BU + 512] = w_in[:, :, bu]
    wfm[:, :, O_CQ:O_CQ + 512] = w_in[:, :, cq]
    wfm[:, :, O_CK:O_CK + 512] = w_in[:, :, ck]
    wfm[:, :, O_DQK:O_DQK + 1024] = w_in[:, :, dqk]
    wfm[:, :, O_DO:O_DO + 512] = w_in[:, :, do]
    wfm[:, :, O_G:O_G + 4] = w_in[:, :, di]
    wfm[:, :, O_G + 4:O_G + 8] = w_in[:, :, df]
    wtm = np.concatenate([w_in[:, :, av], w_in[:, :, cv], w_in[:, :, dv], w_in[:, :, aiw]], axis=2)
    m = {
        "xT": xT if xT is not None else np.ascontiguousarray(f(inp["x"])[b].T),
        "normg": np.stack([np.stack([_col(inp["norm_g"][l, j]) for j in range(3)]) for l in range(DEPTH)]),
        "finalg": _col(inp["final_g"]),
        "bgate": np.stack([np.stack([_col(inp["b_gate"][l, n]) for n in range(4)]) for l in range(DEPTH)]),
        "ident": np.eye(128, dtype=np.float32),
    }
    for l in range(DEPTH):
        m["wffi%d" % l] = f(inp["w_ffn_in"][l])
        m["wffo%d" % l] = f(inp["w_ffn_out"][l])
        m["wfm%d" % l] = np.ascontiguousarray(wfm[l])
        m["wtm%d" % l] = np.ascontiguousarray(wtm[l])
        m["wgate%d" % l] = f(inp["w_gate"][l])
        m["wbr%d" % l] = f(inp["w_branch"][l])
        m["wout%d" % l] = f(inp["w_out"][l])
    rel = f(inp["rel_table"])
    tl = np.arange(128)[:, None]
    j = np.arange(256)[None, :]
    dist = tl + 128 - j
    bk = _rel_bucket(np.maximum(dist, 0))
    nb = np.transpose(rel[bk], (2, 0, 1)).copy()
    nb[:, dist < 0] = -30000.0
    m["nbias"] = np.ascontiguousarray(nb, dtype=np.float32)
    m["c31"] = np.ascontiguousarray(np.broadcast_to(rel[31][None, :], (128, 8)))
    up = (np.arange(128)[None, :] > np.arange(128)[:, None])
    m["cneg"] = np.where(up, -1e30, 0.0).astype(np.float32)
    m["cpos"] = np.where(up, 2e30, 0.0).astype(np.float32)
    m["p2tab"] = np.ascontiguousarray(np.broadcast_to((0.5 ** np.arange(1, NITER + 1))[None, :], (128, NITER)), dtype=np.float32)
    m["sublng"] = np.ascontiguousarray(np.broadcast_to(f(inp["diff_subln_g"])[:, None, :], (DEPTH, 128, 128)))
    def pk(a):
        return np.ascontiguousarray(a.reshape(DEPTH, 16, 128).transpose(0, 2, 1))
    m["s5ar"] = pk(f(inp["s5_a_re"]))
    m["s5ai"] = pk(f(inp["s5_a_im"]))
    m["s5ldt"] = pk(np.broadcast_to(f(inp["s5_log_dt"])[:, :, None], (DEPTH, 32, 64)))
    bre, bim = f(inp["s5_b_re"]), f(inp["s5_b_im"])
    cre, cim = f(inp["s5_c_re"]), f(inp["s5_c_im"])
    s5b = np.zeros((DEPTH, 16, 2, 128, 128), np.float32)
    s5c = np.zeros((DEPTH, 16, 2, 128, 128), np.float32)
    for k in range(16):
        for two in range(2):
            g = 2 * k + two
            ch0 = (k % 4) * 32 + two * 16
            st0 = two * 64
            s5b[:, k, 0, ch0:ch0 + 16, st0:st0 + 64] = bre[:, g].transpose(0, 2, 1)
            s5b[:, k, 1, ch0:ch0 + 16, st0:st0 + 64] = bim[:, g].transpose(0, 2, 1)
            s5c[:, k, 0, st0:st0 + 64, ch0:ch0 + 16] = cre[:, g].transpose(0, 2, 1)
            s5c[:, k, 1, st0:st0 + 64, ch0:ch0 + 16] = cim[:, g].transpose(0, 2, 1)
    m["s5b"] = s5b.reshape(DEPTH, 32, 128, 128)
    m["s5c"] = s5c.reshape(DEPTH, 32, 128, 128)
    m["s5d"] = np.ascontiguousarray(f(inp["s5_d"]).reshape(DEPTH, 4, 128).transpose(0, 2, 1))
    m["s5wglu"] = f(inp["s5_w_glu"])
    m["iota"] = np.ascontiguousarray(np.broadcast_to(np.arange(512, dtype=np.float32)[None, :], (128, 512)))
    m["convw"] = np.ascontiguousarray(f(inp["conv_w"]).reshape(DEPTH, 4, 8, 128).transpose(0, 3, 2, 1))
    mb = np.concatenate([f(inp["mlstm_i_bias"]), f(inp["mlstm_f_bias"])], axis=1)
    m["mbias"] = np.ascontiguousarray(np.broadcast_to(mb[:, None, :], (DEPTH, 128, 8)))
    m["ctri"] = (np.arange(128)[None, :] >= np.arange(128)[:, None]).astype(np.float32)
    m["dlam"] = np.ascontiguousarray(np.broadcast_to(f(inp["diff_lambda"]).reshape(DEPTH, 1, 256), (DEPTH, 128, 256)))
    return m


def _rel_bucket(n):
    n = np.asarray(n)
    large = 16 + (np.log(np.maximum(n, 1).astype(np.float32) / np.float32(16)) / np.float32(math.log(8.0))
                  * np.float32(16)).astype(np.int32)
    large = np.minimum(large, 31)
    return np.where(n < 16, n, large)


_CACHE = {}


def _lamc(l0, n):
    out = np.zeros((max(n, 1), 128, 2), np.float32)
    for i in range(n):
        li = 0.8 - 0.6 * math.exp(-0.3 * (l0 + i))
        out[i, :, 0] = -li
        out[i, :, 1] = 1.0 - li
    return out


def run(inp, S, DEPTH, B, dbg=None):
    key = (S, DEPTH, dbg, "full")
    if key not in _CACHE:
        _CACHE[key] = build(S, DEPTH, dbg)
    nc = _CACHE[key]
    in_maps = [prep_inputs(inp, S, DEPTH, b) for b in range(B)]
    for m in in_maps:
        m["lamc"] = _lamc(0, DEPTH)
    res = run_bass_kernel_spmd(nc, in_maps, core_ids=list(range(B)))
    if dbg is not None:
        _CACHE["dbgy"] = [res.results[b]["dbgy"] for b in range(B)]
    out = np.stack([np.ascontiguousarray(res.results[b]["outT"].T) for b in range(B)])
    return out.astype(np.float32)


_PER_LAYER = ("norm_g", "w_ffn_in", "w_ffn_out", "w_in", "conv_w", "mlstm_i_bias", "mlstm_f_bias", "s5_a_re", "s5_a_im",
              "s5_log_dt", "s5_b_re", "s5_b_im", "s5_c_re", "s5_c_im", "s5_d", "s5_w_glu", "diff_lambda", "diff_subln_g",
              "w_gate", "b_gate", "w_branch", "w_out")


def run_layers(inp, S, DEPTH, B):
    if (S, "layer") not in _CACHE:
        _CACHE[(S, "layer")] = build(S, 1, None, mode="layer")
        _CACHE[(S, "final")] = build(S, 0, None)
    xs = [None] * B
    for l in range(DEPTH):
        sub = {k: (np.asarray(v)[l:l + 1] if k in _PER_LAYER else v) for k, v in inp.items()}
        in_maps = []
        for b in range(B):
            m = prep_inputs(sub, S, 1, b, xT=xs[b])
            m["lamc"] = _lamc(l, 1)
            in_maps.append(m)
        res = run_bass_kernel_spmd(_CACHE[(S, "layer")], in_maps, core_ids=list(range(B)))
        xs = [np.ascontiguousarray(res.results[b]["outT"]) for b in range(B)]
        del in_maps, res
    sub = {k: (np.asarray(v)[0:1] if k in _PER_LAYER else v) for k, v in inp.items()}
    in_maps = []
    for b in range(B):
        m = prep_inputs(sub, S, 1, b, xT=xs[b])
        m = {k: v for k, v in m.items() if not (k[-1] == "0" and k[:-1] in ("wffi", "wffo", "wfm", "wtm", "wgate", "wbr", "wout"))}
        m["lamc"] = _lamc(0, 0)
        in_maps.append(m)
    res = run_bass_kernel_spmd(_CACHE[(S, "final")], in_maps, core_ids=list(range(B)))
    out = np.stack([np.ascontiguousarray(res.results[b]["outT"].T) for b in range(B)])
    return out.astype(np.float32)


def kernel(**inputs):
    x = np.asarray(inputs["x"])
    B, S, _ = x.shape
    DEPTH = np.asarray(inputs["norm_g"]).shape[0]
    return run_layers(inputs, S, DEPTH, B)
```
